# Optimizing a Trainium2 kernel written in Bass

```python
import math
import jax, jax.numpy as jnp
from jax import lax
import numpy as np

D_MODEL = 1024
BATCH = 32
SEQ = 2048
DEPTH = 1

N_HEADS = 16
N_KV_HEADS = 2
GROUP = N_HEADS // N_KV_HEADS
HEAD_DIM = 64
WINDOW = 128
BLOCK = 128
ATTN_SCALE = 1.0 / math.sqrt(HEAD_DIM)
NEG_INF = -1e30
POOL_WINDOWS = (2, 4, 8, 16)
N_POOL_GROUPS = len(POOL_WINDOWS)
POOL_GROUP = 128
POOL_W = N_POOL_GROUPS * POOL_GROUP
Q_W = N_HEADS * HEAD_DIM
KV_W = N_KV_HEADS * HEAD_DIM
IN_W = Q_W + 2 * KV_W + POOL_W + 2 * D_MODEL
SPLITS = (Q_W, Q_W + KV_W, Q_W + 2 * KV_W, Q_W + 2 * KV_W + POOL_W, Q_W + 2 * KV_W + POOL_W + D_MODEL)
PEER_HEADS = 8
N_KEYS = 128
N_EXPERTS = N_KEYS * N_KEYS
D_KEY = 256
D_HALF = D_KEY // 2
PEER_TOPK = 16
PEER_CHUNK = 128
EPS = 1e-5

kernel_name = 'hybrid_swa_pool_peer_block'


def rmsnorm(x, g):
    xf = x.astype(jnp.float32)
    y = xf * lax.rsqrt(jnp.mean(xf * xf, axis=-1, keepdims=True) + EPS)
    return (y * g.astype(jnp.float32)).astype(x.dtype)


def sliding_window_attention(q, k, v, sinks):
    b, s = q.shape[0], q.shape[1]
    nblk = s // BLOCK
    kp = jnp.pad(k, ((0, 0), (BLOCK, 0), (0, 0), (0, 0)))
    vp = jnp.pad(v, ((0, 0), (BLOCK, 0), (0, 0), (0, 0)))
    qb = q.reshape(b, nblk, BLOCK, N_KV_HEADS, GROUP, HEAD_DIM).swapaxes(0, 1)
    sink = sinks.astype(jnp.float32).reshape(1, N_KV_HEADS, GROUP, 1, 1)

    def one_block(args):
        i, qi = args
        start = i * BLOCK
        kb = lax.dynamic_slice_in_dim(kp, start, 2 * BLOCK, axis=1)
        vb = lax.dynamic_slice_in_dim(vp, start, 2 * BLOCK, axis=1)
        sc = jnp.einsum('bqhgd,bkhd->bhgqk', qi, kb).astype(jnp.float32) * ATTN_SCALE
        qpos = start + jnp.arange(BLOCK)
        kpos = start - BLOCK + jnp.arange(2 * BLOCK)
        rel = qpos[:, None] - kpos[None, :]
        mask = (rel >= 0) & (rel < WINDOW) & (kpos[None, :] >= 0)
        sc = jnp.where(mask, sc, NEG_INF)
        sc = jnp.concatenate([sc, jnp.broadcast_to(sink, sc.shape[:-1] + (1,))], axis=-1)
        p = jax.nn.softmax(sc, axis=-1)[..., :-1]
        return jnp.einsum('bhgqk,bkhd->bqhgd', p.astype(vb.dtype), vb)

    o = lax.map(one_block, (jnp.arange(nblk), qb))
    return o.swapaxes(0, 1).reshape(b, s, Q_W)


def multiscale_pool(p, w_grp, scale):
    b, s = p.shape[0], p.shape[1]
    pf = p.astype(jnp.float32).reshape(b, s, N_POOL_GROUPS, POOL_GROUP)
    c = jnp.cumsum(pf, axis=1)
    t1 = jnp.arange(1, s + 1, dtype=jnp.float32)
    outs = []
    for j, w in enumerate(POOL_WINDOWS):
        cj = c[:, :, j]
        lag = jnp.pad(cj, ((0, 0), (w, 0), (0, 0)))[:, :s]
        cnt = jnp.minimum(t1, float(w))[None, :, None]
        outs.append((cj - lag) / cnt - pf[:, :, j])
    pooled = jnp.stack(outs, axis=2).astype(p.dtype)
    y = jnp.einsum('bsgc,gcd->bsgd', pooled, w_grp).reshape(b, s, POOL_W)
    return y * scale


def peer(xn, w_query, sub_keys, u_tab, v_tab):
    b, s, d = xn.shape
    xt = xn.reshape((b * s) // PEER_CHUNK, PEER_CHUNK, d)

    def chunk(xc):
        c = xc.shape[0]
        q = (xc @ w_query).reshape(c, PEER_HEADS, 2, D_HALF)
        sc = jnp.einsum('chpd,hpkd->chpk', q, sub_keys).astype(jnp.float32)
        sv, si = lax.top_k(sc, PEER_TOPK)
        cand = (sv[:, :, 0, :, None] + sv[:, :, 1, None, :]).reshape(c, PEER_HEADS, PEER_TOPK * PEER_TOPK)
        cidx = (si[:, :, 0, :, None] * N_KEYS + si[:, :, 1, None, :]).reshape(c, PEER_HEADS, PEER_TOPK * PEER_TOPK)
        fv, fi = lax.top_k(cand, PEER_TOPK)
        idx = jnp.take_along_axis(cidx, fi, axis=-1)
        g = jax.nn.softmax(fv, axis=-1)
        u = u_tab[idx]
        a = jax.nn.gelu(jnp.einsum('chkd,cd->chk', u, xc).astype(jnp.float32), approximate=False)
        vv = v_tab[idx]
        return jnp.einsum('chk,chkd->cd', (g * a).astype(vv.dtype), vv)

    return lax.map(chunk, xt).reshape(b, s, d)


def setup_inputs(seed: int = 0) -> dict:
    key = jax.random.key(seed)
    ks = jax.random.split(key, 16)
    f32 = jnp.float32
    L = DEPTH
    nrm = lambda k, shape, std: jax.random.normal(k, shape, f32) * std
    return {
        'x': jax.random.normal(ks[0], (BATCH, SEQ, D_MODEL), f32),
        'ln_mix_g': 1.0 + nrm(ks[1], (L, D_MODEL), 0.05),
        'w_in': nrm(ks[2], (L, D_MODEL, IN_W), D_MODEL ** -0.5),
        'b_in': nrm(ks[3], (L, IN_W), 0.02),
        'attn_sinks': nrm(ks[4], (L, N_HEADS), 0.5),
        'w_attn_up': nrm(ks[5], (L, Q_W, D_MODEL), Q_W ** -0.5),
        'w_pool_grp': nrm(ks[6], (L, N_POOL_GROUPS, POOL_GROUP, POOL_GROUP), POOL_GROUP ** -0.5),
        'pool_scale': 1.0 + nrm(ks[7], (L, POOL_W), 0.1),
        'w_pool_up': nrm(ks[8], (L, POOL_W, D_MODEL), POOL_W ** -0.5),
        'w_o': nrm(ks[9], (L, D_MODEL, D_MODEL), D_MODEL ** -0.5),
        'ln_ffn_g': 1.0 + nrm(ks[10], (L, D_MODEL), 0.05),
        'w_query': nrm(ks[11], (L, D_MODEL, PEER_HEADS * D_KEY), D_MODEL ** -0.5),
        'sub_keys': nrm(ks[12], (L, PEER_HEADS, 2, N_KEYS, D_HALF), D_HALF ** -0.5),
        'u_experts': nrm(ks[13], (L, N_EXPERTS, D_MODEL), D_MODEL ** -0.5),
        'v_experts': nrm(ks[14], (L, N_EXPERTS, D_MODEL), PEER_HEADS ** -0.5),
        'ln_final_g': 1.0 + nrm(ks[15], (D_MODEL,), 0.05),
    }


def reference(x, ln_mix_g, w_in, b_in, attn_sinks, w_attn_up, w_pool_grp, pool_scale, w_pool_up, w_o,
              ln_ffn_g, w_query, sub_keys, u_experts, v_experts, ln_final_g):
    b, s, _ = x.shape
    h = x
    for l in range(DEPTH):
        xn = rmsnorm(h, ln_mix_g[l])
        z = xn @ w_in[l] + b_in[l]
        q, k, v, pz, ga, gp = jnp.split(z, SPLITS, axis=-1)
        q = q.reshape(b, s, N_KV_HEADS, GROUP, HEAD_DIM)
        k = k.reshape(b, s, N_KV_HEADS, HEAD_DIM)
        v = v.reshape(b, s, N_KV_HEADS, HEAD_DIM)
        y_a = sliding_window_attention(q, k, v, attn_sinks[l]) @ w_attn_up[l]
        y_p = multiscale_pool(pz, w_pool_grp[l], pool_scale[l]) @ w_pool_up[l]
        merged = jax.nn.sigmoid(ga) * y_a + jax.nn.sigmoid(gp) * y_p
        h = h + merged @ w_o[l]
        h = h + peer(rmsnorm(h, ln_ffn_g[l]), w_query[l], sub_keys[l], u_experts[l], v_experts[l])
    return rmsnorm(h, ln_final_g)
```

```python
import numpy as np
from contextlib import ExitStack
import concourse.bass as bass
import concourse.mybir as mybir
from concourse.bass_utils import run_bass_kernel_spmd

F32 = mybir.dt.float32
BF16 = mybir.dt.bfloat16
U32 = mybir.dt.uint32
AF = mybir.ActivationFunctionType
ALU = mybir.AluOpType
AX = mybir.AxisListType

N_CORES = 8
T = 256
NCH_FULL = 32
EPS = 1e-5
NEG = -1e30

B_WIN, B_AUP, B_PUP, B_WO, B_WQ, B_U, B_V, B_GRP, B_SK = 0, 32, 40, 44, 52, 68, 196, 324, 325
NBLK = 327
C_G, C_B, C_SINK, C_PSC, C_ID, C_ONES, C_IOTA, C_MREST, C_MFIRST, C_OLO, C_OHI, C_RC = (
    0, 24, 56, 64, 68, 196, 324, 452, 964, 1476, 1604, 1732)
NCST = 1796


class _Op:
    __slots__ = ("eng", "emit", "deps", "need_inc", "ms", "dma_sem")

    def __init__(self, eng, emit, dma_sem=None):
        self.eng = eng
        self.emit = emit
        self.deps = []
        self.need_inc = False
        self.ms = None
        self.dma_sem = dma_sem


class Sched:
    ENGS = ("pe", "act", "dve", "pool", "sp")

    def __init__(self):
        self.ops = {e: [] for e in self.ENGS}
        self.last_writer = {}
        self.readers = {}
        self.dma_sems = []
        self.bank_acc = {}

    def add(self, eng, emit, reads=(), writes=(), after=(), dma_sem=None, psum=()):
        op = _Op(eng, emit, dma_sem)
        deps = []
        for b in psum:
            acc = self.bank_acc.setdefault(b, {})
            for e2, op2 in acc.items():
                if e2 != eng:
                    deps.append(op2)
            acc[eng] = op
        for k in reads:
            w = self.last_writer.get(k)
            if w is not None:
                deps.append(w)
        for k in tuple(writes) + tuple(after):
            w = self.last_writer.get(k)
            if w is not None:
                deps.append(w)
            deps.extend(self.readers.get(k, ()))
        seen = set()
        for d in deps:
            if id(d) in seen:
                continue
            seen.add(id(d))
            if d.eng == "pe" and eng == "pe":
                continue
            op.deps.append(d)
            d.need_inc = True
        for k in reads:
            self.readers.setdefault(k, []).append(op)
        for k in writes:
            self.last_writer[k] = op
            self.readers[k] = []
        if dma_sem is not None and dma_sem not in self.dma_sems:
            self.dma_sems.append(dma_sem)
        self.ops[eng].append(op)
        return op

    def finalize(self):
        cnt = {e: 0 for e in self.ENGS}
        dcnt = {}
        for e in self.ENGS:
            for op in self.ops[e]:
                if op.dma_sem is not None:
                    dcnt[op.dma_sem] = dcnt.get(op.dma_sem, 0) + 16
                    op.ms = (("dma", op.dma_sem), dcnt[op.dma_sem])
                elif op.need_inc:
                    cnt[e] += 1
                    op.ms = (("eng", e), cnt[e])
        return cnt

    def emit_engine(self, e, eng, sems):
        waited = {}
        for op in self.ops[e]:
            need = {}
            for d in op.deps:
                k, v = d.ms
                if waited.get(k, 0) >= v:
                    continue
                if need.get(k, 0) < v:
                    need[k] = v
            for k, v in need.items():
                eng.wait_ge(sems[k], v)
                waited[k] = v
            ins = op.emit(eng)
            if op.ms is not None:
                k, v = op.ms
                ins.then_inc(sems[k], 16 if k[0] == "dma" else 1)


class _DQ:
    def __init__(self):
        self.l = []

    def add(self, *a, **k):
        self.l.append((a, k))

    def replay(self, S, n=None):
        m = len(self.l) if n is None else min(n, len(self.l))
        for a, k in self.l[:m]:
            S.add(*a, **k)
        del self.l[:m]


def _mm(out, lhsT, rhs, start=True, stop=True, sgc=False):
    if sgc:
        return lambda e: e.matmul(out=out, lhsT=lhsT, rhs=rhs, start=start, stop=stop, skip_group_check=True)
    return lambda e: e.matmul(out=out, lhsT=lhsT, rhs=rhs, start=start, stop=stop)


def _tr(out, in_, ident):
    return lambda e: e.transpose(out=out, in_=in_, identity=ident)


def _act(out, in_, func, bias=None, scale=None):
    kw = {}
    if bias is not None:
        kw["bias"] = bias
    if scale is not None:
        kw["scale"] = scale
    return lambda e: e.activation(out=out, in_=in_, func=func, **kw)


def _tt(out, in0, in1, op):
    return lambda e: e.tensor_tensor(out=out, in0=in0, in1=in1, op=op)


def _ts(out, in0, s1, op0, s2=None, op1=None):
    if op1 is None:
        return lambda e: e.tensor_scalar(out=out, in0=in0, scalar1=s1, scalar2=None, op0=op0)
    return lambda e: e.tensor_scalar(out=out, in0=in0, scalar1=s1, scalar2=s2, op0=op0, op1=op1)


def _stt(out, in0, scalar, in1, op0, op1):
    return lambda e: e.scalar_tensor_tensor(out=out, in0=in0, scalar=scalar, in1=in1, op0=op0, op1=op1)


def _cp(out, in_):
    return lambda e: e.tensor_copy(out=out, in_=in_)


def _dma(out, in_):
    return lambda e: e.dma_start(out=out, in_=in_)


class _Stop(Exception):
    pass


def build_nc(n_chunks=NCH_FULL, dbg=False, stage=99):
    nc = bass.Bass("TRN2", target_bir_lowering=False)
    xd = nc.dram_tensor("xd", [n_chunks, 128, 8, T], F32, kind="ExternalInput").ap()
    wf = nc.dram_tensor("wf", [NBLK, 128, 1024], F32, kind="ExternalInput").ap()
    cstd = nc.dram_tensor("cst", [128, NCST], F32, kind="ExternalInput").ap()
    wb = nc.dram_tensor("wb", [NBLK, 128, 1024], BF16, kind="Internal").ap()
    od = nc.dram_tensor("od", [n_chunks, 128, 8, T], F32, kind="ExternalOutput").ap()
    if dbg:
        d_h2 = nc.dram_tensor("d_h2", [n_chunks, 128, 8, T], F32, kind="ExternalOutput").ap()
        d_sel = nc.dram_tensor("d_sel", [n_chunks, 2, 128, 384], F32, kind="ExternalOutput").ap()
        d_h3 = nc.dram_tensor("d_h3", [n_chunks, 128, 8, T], F32, kind="ExternalOutput").ap()

    S = Sched()
    es = ExitStack()
    with es:
        def sb(name, shape, dt):
            return es.enter_context(nc.sbuf_tensor(name, shape, dt))

        NB = 7
        xT = [sb("xT%d" % i_, [128, 8, T], F32) for i_ in range(3)]
        uni = sb("uni", [128, 16384], F32)
        unib = uni[:].bitcast(BF16)

        def uf(off, n):
            return uni[:, off // 4: off // 4 + n]

        def ub(off, n):
            return unib[:, off // 2: off // 2 + n]

        sga = uf(0, 2048).rearrange("p (m t) -> p m t", m=8)
        sgp = uf(8192, 2048).rearrange("p (m t) -> p m t", m=8)
        tmpA = uf(16384, 2048).rearrange("p (m t) -> p m t", m=8)
        sq = uf(24576, 2048).rearrange("p (m t) -> p m t", m=8)
        xnT = ub(32768, 2048).rearrange("p (m t) -> p m t", m=8)
        mergedT = ub(36864, 2048).rearrange("p (m t) -> p m t", m=8)
        qT = ub(40960, 2048).rearrange("p (m t) -> p m t", m=8)
        attnT = ub(45056, 2048).rearrange("p (m t) -> p m t", m=8)
        Ebuf = ub(49152, 2048).rearrange("p (m t) -> p m t", m=4)
        PTb = ub(53248, 2048).rearrange("p (m t) -> p m t", m=4)
        rd = uf(57344, 512).rearrange("p (m t) -> p m t", m=2)
        tmpB = uf(59392, 512).rearrange("p (m t) -> p m t", m=2)
        G = unib.rearrange("p (t j) -> p t j", j=128)
        NPS = 4
        pst_f = [uf(i_ * 8192, 2048) for i_ in range(NPS)]
        pst_b = [ub(32768 + i_ * 4096, 2048) for i_ in range(NPS)]
        PSTK = ["pstf%d" % i_ for i_ in range(NPS)] + ["pstb%d" % i_ for i_ in range(NPS)]
        UNI_KEYS = ["sga", "sgp", "tmpA", "sq", "xnT", "mergedT", "qT", "attnT", "E0", "E1", "E2", "E3",
                    "PT0", "PT1", "PT2", "PT3", "rd0", "rd1", "tmpB0", "tmpB1"] + PSTK
        for m in range(8):
            UNI_KEYS += [("sga", m), ("sgp", m), ("tmpA", m), ("sq", m), ("xnT", m), ("mergedT", m), ("qT", m),
                         ("attnT", m)]
        GK = ["G"] + PSTK

        xn2 = [sb("xn2Ta", [128, 8, T], BF16), sb("xn2Tb", [128, 8, T], BF16)]
        rstd = sb("rstd", [128, T], F32)
        kT = sb("kT", [128, 2, 2, 384], BF16)
        vT = sb("vT", [128, 2, T], BF16)
        vlo = sb("vlo", [128, 2, 2, 3, 128], BF16)
        vhi = sb("vhi", [128, 2, 2, 3, 128], BF16)
        pz = sb("pz", [128, 2, 4, 272], F32)
        ptmp = sb("ptmp", [128, 4, 272], F32)
        pfix = sb("pfix", [128, 16], F32)
        pooledT = sb("pooledT", [128, 4, T], BF16)
        ypT = sb("ypT", [128, 4, T], BF16)
        qpT = sb("qpT", [128, 16, T], BF16)
        sc = sb("sc", [128, 2048], F32)
        sc2 = sb("sc2", [128, 2048], F32)
        sv = sb("sv", [128, 16, 16], F32)
        si = sb("si", [128, 16, 16], U32)
        sif = sb("sif", [128, 16, 16], F32)
        fv = sb("fv", [128, 8, 16], F32)
        fi = sb("fi", [128, 8, 16], U32)
        fsh = sb("fsh", [128, 8, 16], F32)
        ex = sb("ex", [128, 8, 16], F32)
        ssum = sb("ssum", [128, 8], F32)
        sel3 = sb("sel3", [128, 2, 384], F32)
        au = sb("au", [128, 8, 16], U32)
        bu = sb("bu", [128, 8, 16], U32)
        af = sb("af", [128, 8, 16], F32)
        bf = sb("bf", [128, 8, 16], F32)
        eq = sb("eq", [128, 2048], F32)
        selT = sb("selT", [128, 2, 384], F32)
        NOH = 8
        oh0 = sb("oh0", [128, NOH, 128], BF16)
        oh1 = sb("oh1", [128, NOH, 128], BF16)
        wt = sb("wt", [128, 4, T], BF16)
        ge = sb("ge", [128, 4, T], BF16)
        ring = sb("ring", [128, NB, 1024], BF16)
        cst = sb("cstt", [128, NCST], F32)
        maskb = sb("maskb", [128, 1024], BF16)
        oneslh = sb("oneslh", [128, 256], BF16)
        identb = sb("identb", [128, 128], BF16)
        iota_b = sb("iota_b", [128, 128], BF16)
        wgrp_b = sb("wgrp_b", [128, 512], BF16)
        skT_b = sb("skT_b", [128, 2048], BF16)
        sinkexp = sb("sinkexp", [128, 8], F32)
        bhalf = sb("bhalf", [128, 16], F32)
        negj = sb("negj", [128, 2, 128], F32)

        pb = [es.enter_context(nc.psum_tensor("pb%d" % i, [128, 512], F32)) for i in range(8)]

        def PK(bank, half=None):
            if half is None:
                return [("ps", bank, 0), ("ps", bank, 1)]
            return [("ps", bank, half)]

        ident_f = cst[:, C_ID:C_ID + 128]
        ones_f = cst[:, C_ONES:C_ONES + 128]
        iota_f = cst[:, C_IOTA:C_IOTA + 128]

        S.add("sp", _dma(cst[:], cstd), writes=["cst"], dma_sem="cst")
        S.add("dve", _cp(maskb[:], cst[:, C_MREST:C_MREST + 1024]), reads=["cst"], writes=["maskb"])
        S.add("dve", _cp(oneslh[:], cst[:, C_OLO:C_OLO + 256]), reads=["cst"], writes=["oneslh"])
        S.add("dve", _cp(identb[:], ident_f), reads=["cst"], writes=["identb"])
        S.add("dve", _cp(iota_b[:], iota_f), reads=["cst"], writes=["iota_b"])
        S.add("act", _act(sinkexp[:], cst[:, C_SINK:C_SINK + 8], AF.Exp), reads=["cst"], writes=["sinkexp"])
        S.add("dve", _ts(bhalf[:], cst[:, C_B + 16:C_B + 32], 0.5, ALU.mult), reads=["cst"], writes=["bhalf"])
        S.add("pool", lambda e: e.memset(kT[:], 0.0), writes=["kTc0", "kTc1"])
        S.add("pool", lambda e: e.memset(vlo[:], 0.0), writes=["vc0", "vc1"])
        S.add("pool", lambda e: e.memset(vhi[:], 0.0), writes=["vc0", "vc1"])
        S.add("pool", lambda e: e.memset(ptmp[:], 0.0), writes=[("ptmp", l_) for l_ in range(4)])
        S.add("pool", lambda e: e.memset(pz[:], 0.0), writes=[("pz", p_, g_) for p_ in range(2) for g_ in range(4)])

        cast_engs = ["act", "dve", "pool"]
        ngrp = (NBLK + 1) // 2
        LA = NPS - 1
        stores = []
        for gi in range(ngrp + LA):
            if gi < ngrp:
                b0 = gi * 2
                nb = min(2, NBLK - b0)
                s_ = gi % NPS
                src = wf[b0:b0 + nb].rearrange("b p n -> p b n")
                dst = wb[b0:b0 + nb].rearrange("b p n -> p b n")
                fview = pst_f[s_][:, 0:nb * 1024].rearrange("p (b n) -> p b n", b=nb)
                bview = pst_b[s_][:, 0:nb * 1024].rearrange("p (b n) -> p b n", b=nb)
                S.add("sp", _dma(fview, src), writes=["pstf%d" % s_], dma_sem=("pl", s_))
                ce = cast_engs[gi % 3]
                if ce == "act":
                    S.add("act", _act(bview, fview, AF.Copy), reads=["pstf%d" % s_], writes=["pstb%d" % s_])
                else:
                    S.add(ce, _cp(bview, fview), reads=["pstf%d" % s_], writes=["pstb%d" % s_])
                stores.append((dst, bview, s_, b0, nb))
            if gi >= LA:
                dst, bview, s_, b0, nb = stores[gi - LA]
                S.add("sp", _dma(dst, bview), reads=["pstb%d" % s_], writes=[("scr", b0 + i) for i in range(nb)],
                      dma_sem=("pst", s_))
        S.add("sp", _dma(wgrp_b[:], wb[B_GRP, :, 0:512]), reads=[("scr", B_GRP)], writes=["wgrp_b"], dma_sem="rw0")
        S.add("sp", _dma(skT_b[:].rearrange("p (b n) -> p b n", b=2), wb[B_SK:B_SK + 2].rearrange("b p n -> p b n")),
              reads=[("scr", B_SK), ("scr", B_SK + 1)], writes=["skT_b"], dma_sem="rw1")

        ring_state = {"n": 0}

        def ring_next(blk):
            slot = ring_state["n"] % NB
            ring_state["n"] += 1
            key = ("ring", slot)
            S.add("sp", _dma(ring[:, slot, :], wb[blk]), reads=[("scr", blk)], writes=[key], dma_sem=("w", slot))
            return ring[:, slot, :], key

        mm_state = {}

        def mm_slot(banks=(4, 5)):
            i = mm_state.get(banks, 0)
            mm_state[banks] = i + 1
            bank = banks[i % len(banks)]
            return pb[bank][:, 0:256], bank

        sq_uni = (sq, lambda kc: [("sq", kc)], GK, ())
        sq_sc2 = (sc2[:].rearrange("p (m t) -> p m t", m=8), lambda kc: [("sc2", 2 * kc), ("sc2", 2 * kc + 1)], (), ())
        sq_eq = (eq[:].rearrange("p (m t) -> p m t", m=8), lambda kc: [("eqn", kc)], ("eq",), ("eq",))
        rs_std = (rstd, "rstd")
        rs_sif = (sif[:].rearrange("p a b -> p (a b)"), "sif")

        def rmsnorm_thunks(src, kin, dst, kout, gcol, out_after=(), sqs=None, rs=None):
            sqv, sqk, sq_after, sq_xr = sqs if sqs is not None else sq_uni
            rsv, rsk = rs if rs is not None else rs_std
            slot = {}

            def t1():
                for kc in range(8):
                    S.add("act", _act(sqv[:, kc, :], src[:, kc, :], AF.Square), reads=[(kin, kc)], writes=sqk(kc),
                          after=sq_after)

            def t2():
                ps_, pk = mm_slot()
                for kc in range(8):
                    S.add("pe", _mm(ps_, ones_f, sqv[:, kc, :], start=(kc == 0), stop=(kc == 7)),
                          reads=sqk(kc) + ["cst"] + list(sq_xr), psum=[pk])
                S.add("act", _act(rsv[:], ps_, AF.Sqrt, bias=EPS, scale=1.0 / 1024.0), psum=[pk], writes=[rsk])

            def t3():
                S.add("dve", lambda e: e.reciprocal(out=rsv[:], in_=rsv[:]), reads=[rsk], writes=[rsk])

            def t4():
                for kc in range(8):
                    S.add("dve", _stt(dst[:, kc, :], src[:, kc, :], cst[:, gcol + kc:gcol + kc + 1], rsv[:], ALU.mult,
                                      ALU.mult),
                          reads=[(kin, kc), rsk, "cst"], writes=[(kout, kc)], after=out_after)

            return [t1, t2, t3, t4]

        def rmsnorm(src, kin, dst, kout, gcol, out_after=(), sqs=None, rs=None):
            for t_ in rmsnorm_thunks(src, kin, dst, kout, gcol, out_after, sqs, rs):
                t_()

        S.add("sp", _dma(xT[0][:], xd[0]), writes=[("x0", kc) for kc in range(8)], dma_sem=("x", 0))
        def mixer(n, fin_thunks=()):
            fin_thunks = list(fin_thunks)
            par = n % 2
            npar = 1 - par
            first = (n % 8 == 0)
            X = xT[n % 3]
            xk = "x%d" % (n % 3)
            kTc, kTn = "kTc%d" % par, "kTc%d" % npar
            vc, vn = "vc%d" % par, "vc%d" % npar
            pzc, pzn = "pzc%d" % par, "pzc%d" % npar

            rmsnorm(X, xk, xnT, "xnT", C_G + 0, out_after=GK)

            def inproj_chunk(oc):
                wsl, wk = ring_next(B_WIN + oc)
                w3 = wsl.rearrange("p (k m) -> p k m", k=8)
                ps_, pk = mm_slot()
                for kc in range(8):
                    S.add("pe", _mm(ps_, w3[:, kc, :], xnT[:, kc, :], start=(kc == 0), stop=(kc == 7)),
                          reads=[wk, ("xnT", kc)], psum=[pk])
                bcol = cst[:, C_B + oc:C_B + oc + 1]
                if oc < 8:
                    S.add("act", _act(qT[:, oc, :], ps_, AF.Identity, bias=bcol), psum=[pk], reads=["cst"],
                          writes=[("qT", oc)], after=GK)
                elif oc < 10:
                    kv = oc - 8
                    S.add("act", _act(kT[:, par, kv, 128:384], ps_, AF.Identity, bias=bcol), psum=[pk], reads=["cst"],
                          writes=[kTc])
                    S.add("act", _act(kT[:, npar, kv, 0:128], ps_[:, 128:256], AF.Identity, bias=bcol),
                          psum=[pk], reads=["cst"], writes=[kTn])
                elif oc < 12:
                    kv = oc - 10
                    S.add("act", _act(vT[:, kv, :], ps_, AF.Identity, bias=bcol), psum=[pk], reads=["cst"],
                          writes=[("vT", kv)])
                    p3b = pb[3][:].bitcast(BF16)
                    for blk in range(2):
                        qi = kv * 2 + blk
                        o_ = p3b[:, qi * 256:qi * 256 + 128]
                        S.add("pe", _tr(o_, vT[:, kv, blk * 128:(blk + 1) * 128], identb[:]),
                              reads=[("vT", kv), "identb"], psum=[3])
                        S.add("dve", _cp(vlo[:, par, kv, blk + 1, 0:64], o_[:, 0:64]), psum=[3], writes=[vc])
                        S.add("dve", _cp(vhi[:, par, kv, blk + 1, 64:128], o_[:, 64:128]), psum=[3], writes=[vc])
                        if blk == 1:
                            S.add("dve", _cp(vlo[:, npar, kv, 0, 0:64], o_[:, 0:64]), psum=[3], writes=[vn])
                            S.add("dve", _cp(vhi[:, npar, kv, 0, 64:128], o_[:, 64:128]), psum=[3], writes=[vn])
                elif oc < 16:
                    g = oc - 12
                    if first:
                        S.add("pool", lambda e, g=g: e.memset(pz[:, par, g, 0:16], 0.0), writes=[("pz", par, g)])
                    S.add("act", _act(pz[:, par, g, 16:272], ps_, AF.Identity, bias=bcol), psum=[pk], reads=["cst"],
                          writes=[("pz", par, g)])
                    S.add("act", _act(pz[:, npar, g, 0:16], ps_[:, 240:256], AF.Identity, bias=bcol),
                          psum=[pk], reads=["cst"], writes=[("pz", npar, g)])
                elif oc < 24:
                    m = oc - 16
                    S.add("act", _act(sga[:, m, :], ps_, AF.Tanh, bias=bhalf[:, m:m + 1], scale=0.5), psum=[pk],
                          reads=["bhalf"], writes=[("sga", m)], after=GK)
                else:
                    m = oc - 24
                    S.add("act", _act(sgp[:, m, :], ps_, AF.Tanh, bias=bhalf[:, 8 + m:9 + m], scale=0.5), psum=[pk],
                          reads=["bhalf"], writes=[("sgp", m)], after=GK)

            for oc_ in range(12):
                inproj_chunk(oc_)
                if fin_thunks and oc_ % 2 == 0:
                    fin_thunks.pop(0)()
            while fin_thunks:
                fin_thunks.pop(0)()
            f_pool = [(lambda oc_=oc_: inproj_chunk(oc_)) for oc_ in range(12, 16)]

            def pool_group(g):
                w = 2 << g
                p_ = pz[:, par, g, :]
                cur = p_
                curk = [("pz", par, g)]
                sh = 1
                lvl = 0
                while sh < w:
                    dst = ptmp[:, lvl, :]
                    S.add("pool", _tt(dst[:, sh:272], cur[:, sh:272], cur[:, 0:272 - sh], ALU.add),
                          reads=curk, writes=[("ptmp", lvl)])
                    cur = dst
                    curk = [("ptmp", lvl)]
                    sh *= 2
                    lvl += 1
                S.add("dve", _stt(pooledT[:, g, :], cur[:, 16:272], 1.0 / w, p_[:, 16:272], ALU.mult, ALU.subtract),
                      reads=curk + [("pz", par, g)], writes=[("pooledT", g)])
                if first:
                    S.add("dve", _tt(pfix[:], cur[:, 16:32], cst[:, C_RC + g * 16:C_RC + (g + 1) * 16],
                                     ALU.mult), reads=curk + ["cst"], writes=["pfix"])
                    S.add("dve", _tt(pooledT[:, g, 0:16], pfix[:], p_[:, 16:32], ALU.subtract),
                          reads=["pfix", ("pz", par, g)], writes=[("pooledT", g)])
                ps_, pk = mm_slot()
                S.add("pe", _mm(ps_, wgrp_b[:, g * 128:(g + 1) * 128], pooledT[:, g, :]),
                      reads=["wgrp_b", ("pooledT", g)], psum=[pk])
                S.add("act", _act(ypT[:, g, :], ps_, AF.Identity, scale=cst[:, C_PSC + g:C_PSC + g + 1]),
                      psum=[pk], reads=["cst"], writes=[("ypT", g)])

            f_pool += [(lambda g_=g_: pool_group(g_)) for g_ in range(4)]
            f_gate = [(lambda oc_=oc_: inproj_chunk(oc_)) for oc_ in range(16, 32)]
            fillers = []
            for i_ in range(16):
                fillers.append(f_gate[i_])
                if i_ < 8:
                    fillers.append(f_pool[i_])

            def run_fillers(k):
                for _ in range(k):
                    if fillers:
                        fillers.pop(0)()

            def att_scores(c):
                kv = c // 4
                for hh in range(2):
                    bank = (c % 2) * 2 + hh
                    pr = slice(hh * 64, hh * 64 + 64)
                    ps_ = pb[bank]
                    rk = [("qT", c), kTc]
                    S.add("pe", _mm(ps_[:, 0:128], kT[pr, par, kv, 0:128], qT[pr, c, 0:128], start=True, stop=False,
                                    sgc=True), reads=rk, psum=[bank])
                    S.add("pe", _mm(ps_[:, 128:384], kT[pr, par, kv, 128:256], qT[pr, c, 0:256], start=False,
                                    stop=False, sgc=True), reads=rk, psum=[bank])
                    S.add("pe", _mm(ps_[:, 384:512], kT[pr, par, kv, 256:384], qT[pr, c, 128:256], start=False,
                                    stop=False, sgc=True), reads=rk, psum=[bank])
                    mk = maskb[:, 512:1024] if first else maskb[:, 0:512]
                    S.add("pe", _mm(ps_[:, 0:512], identb[:], mk, start=False, stop=True, sgc=True),
                          reads=["identb", "maskb"], psum=[bank])
                    eb = (c % 2) * 2 + hh
                    S.add("act", _act(PTb[:, eb, :], ps_[:], AF.Exp, scale=0.125), psum=[bank],
                          writes=["PT%d" % eb], after=GK)

            def att_pv(c):
                kv = c // 4
                ob = 6 + (c % 2)
                o_ps = pb[ob][:, 0:256]
                d_ps = pb[ob][:, 256:512]
                for blk in range(2):
                    seq = []
                    for hh in range(2):
                        eb = (c % 2) * 2 + hh
                        vv = vlo if hh == 0 else vhi
                        on = oneslh[:, hh * 128:(hh + 1) * 128]
                        for kb in range(2):
                            col = (blk * 2 + kb) * 128
                            seq.append((vv[:, par, kv, blk + kb, :], on, PTb[:, eb, col:col + 128], "PT%d" % eb))
                    for i_, (vw, on, rhs, rkey) in enumerate(seq):
                        S.add("pe", _mm(o_ps[:, blk * 128:(blk + 1) * 128], vw, rhs, start=(i_ == 0), stop=(i_ == 3)),
                              reads=[rkey, vc], psum=[ob])
                    for i_, (vw, on, rhs, rkey) in enumerate(seq):
                        S.add("pe", _mm(d_ps[:, blk * 128:(blk + 1) * 128], on, rhs, start=(i_ == 0), stop=(i_ == 3)),
                              reads=[rkey, "oneslh"], psum=[ob])
                r_ = c % 2
                S.add("dve", _ts(rd[:, r_, :], d_ps, sinkexp[:, c:c + 1], ALU.add), psum=[ob], reads=["sinkexp"],
                      writes=["rd%d" % r_], after=GK)
                S.add("dve", lambda e, r_=r_: e.reciprocal(out=rd[:, r_, :], in_=rd[:, r_, :]), reads=["rd%d" % r_],
                      writes=["rd%d" % r_])
                S.add("dve", _tt(attnT[:, c, :], o_ps, rd[:, r_, :], ALU.mult), psum=[ob], reads=["rd%d" % r_],
                      writes=[("attnT", c)], after=GK)

            att_scores(0)
            run_fillers(2)
            for c in range(8):
                if c + 1 < 8:
                    att_scores(c + 1)
                    run_fillers(2)
                att_pv(c)
                run_fillers(1)
            run_fillers(100)

            for m in range(8):
                wsl, wk = ring_next(B_AUP + m)
                w3 = wsl.rearrange("p (k m) -> p k m", k=8)
                ps_, pk = mm_slot()
                for kc in range(8):
                    S.add("pe", _mm(ps_, w3[:, kc, :], attnT[:, kc, :], start=(kc == 0), stop=(kc == 7)),
                          reads=[wk, ("attnT", kc)], psum=[pk])
                S.add("dve", _stt(tmpA[:, m, :], sga[:, m, :], 1.0, ps_, ALU.add, ALU.mult), psum=[pk], reads=[("sga", m)],
                      writes=[("tmpA", m)], after=GK)
            for mb in range(4):
                wsl, wk = ring_next(B_PUP + mb)
                w4 = wsl.rearrange("p (i k m) -> p i k m", i=2, k=4)
                for mi in range(2):
                    m = mb * 2 + mi
                    ps_, pk = mm_slot()
                    for kc in range(4):
                        S.add("pe", _mm(ps_, w4[:, mi, kc, :], ypT[:, kc, :], start=(kc == 0), stop=(kc == 3)),
                              reads=[wk, ("ypT", kc)], psum=[pk])
                    tb = m % 2
                    S.add("dve", _stt(tmpB[:, tb, :], sgp[:, m, :], 1.0, ps_, ALU.add, ALU.mult), psum=[pk], reads=[("sgp", m)],
                          writes=["tmpB%d" % tb], after=GK)
                    S.add("pool", _tt(mergedT[:, m, :], tmpB[:, tb, :], tmpA[:, m, :], ALU.add),
                          reads=["tmpB%d" % tb, ("tmpA", m)], writes=[("mergedT", m)], after=GK)
            for m in range(8):
                wsl, wk = ring_next(B_WO + m)
                w3 = wsl.rearrange("p (k m) -> p k m", k=8)
                ps_, pk = mm_slot()
                for kc in range(8):
                    S.add("pe", _mm(ps_, w3[:, kc, :], mergedT[:, kc, :], start=(kc == 0), stop=(kc == 7)),
                          reads=[wk, ("mergedT", kc)], psum=[pk])
                S.add("dve", _stt(X[:, m, :], ps_, 0.5, X[:, m, :], ALU.mult, ALU.add), psum=[pk], reads=[(xk, m)],
                      writes=[(xk, m)])
            if dbg:
                S.add("act", _dma(d_h2[n], X[:]), reads=[(xk, m) for m in range(8)], writes=["d_h2"],
                      dma_sem=("dbg", 0))

        def mixer_tail(n):
            par = n % 2
            X = xT[n % 3]
            xk = "x%d" % (n % 3)
            th = rmsnorm_thunks(X, xk, xn2[par], "xn2T%d" % par, C_G + 8, sqs=sq_sc2)

            def qchunk(oc):
                wsl, wk = ring_next(B_WQ + oc)
                w3 = wsl.rearrange("p (k m) -> p k m", k=8)
                ps_, pk = mm_slot()
                for kc in range(8):
                    S.add("pe", _mm(ps_, w3[:, kc, :], xn2[par][:, kc, :], start=(kc == 0), stop=(kc == 7)),
                          reads=[wk, ("xn2T%d" % par, kc)], psum=[pk])
                S.add("act", _act(qpT[:, oc, :], ps_, AF.Copy), psum=[pk], writes=[("qpT", oc)])

            th += [(lambda oc_=oc_: qchunk(oc_)) for oc_ in range(16)]
            return th

        def topk_q(n):
            QT = [_DQ(), _DQ()]
            QX = [_DQ(), _DQ()]
            for tt in range(2):
                tsl = slice(tt * 128, (tt + 1) * 128)
                Q = QT[tt]
                for b4_ in range(4):
                    bank = 6 + (b4_ % 2)
                    for i_ in range(4):
                        oc = b4_ * 4 + i_
                        Q.add("pe", _mm(pb[bank][:, i_ * 128:(i_ + 1) * 128], qpT[:, oc, tsl],
                                        skT_b[:, oc * 128:(oc + 1) * 128]),
                              reads=[("qpT", oc), "skT_b"], psum=[bank])
                    Q.add("act", _act(sc[:, b4_ * 512:(b4_ + 1) * 512], pb[bank][:], AF.Copy), psum=[bank],
                          writes=[("sc", b4_)])
                for gq in range(16):
                    Q.add("dve", lambda e, gq=gq: e.max(out=sv[:, gq, 0:8], in_=sc[:, gq * 128:(gq + 1) * 128]),
                          reads=[("sc", gq // 4)], writes=[("sv0", gq)])
                for gq in range(16):
                    Q.add("dve", lambda e, gq=gq: e.max_index(out=si[:, gq, 0:8], in_max=sv[:, gq, 0:8],
                                                              in_values=sc[:, gq * 128:(gq + 1) * 128]),
                          reads=[("sc", gq // 4), ("sv0", gq)], writes=[("si0", gq)])
                for gq in range(16):
                    Q.add("dve", lambda e, gq=gq: e.match_replace(out=sc2[:, gq * 128:(gq + 1) * 128],
                                                                  in_to_replace=sv[:, gq, 0:8],
                                                                  in_values=sc[:, gq * 128:(gq + 1) * 128],
                                                                  imm_value=NEG),
                          reads=[("sc", gq // 4), ("sv0", gq)], writes=[("sc2", gq)])
                for gq in range(16):
                    Q.add("dve", lambda e, gq=gq: e.max(out=sv[:, gq, 8:16], in_=sc2[:, gq * 128:(gq + 1) * 128]),
                          reads=[("sc2", gq)], writes=[("sv1", gq)])
                for gq in range(16):
                    Q.add("dve", lambda e, gq=gq: e.max_index(out=si[:, gq, 8:16], in_max=sv[:, gq, 8:16],
                                                              in_values=sc2[:, gq * 128:(gq + 1) * 128]),
                          reads=[("sc2", gq), ("sv1", gq)], writes=[("si1", gq)])
                allsv = [("sv0", gq) for gq in range(16)] + [("sv1", gq) for gq in range(16)]
                allsi = [("si0", gq) for gq in range(16)] + [("si1", gq) for gq in range(16)]
                Q.add("dve", _cp(sif[:], si[:]), reads=allsi, writes=["sif"])
                cand = sc
                sv4 = sv[:].rearrange("p (h two) k -> p h two k", two=2)
                c4 = cand[:].rearrange("p (h a b) -> p h a b", h=8, a=16)
                in0 = sv4[:, :, 0, :].unsqueeze(3).broadcast_to([128, 8, 16, 16])
                in1 = sv4[:, :, 1, :].unsqueeze(2).broadcast_to([128, 8, 16, 16])
                allsc = [("sc", b) for b in range(4)]
                allsc2 = [("sc2", gq) for gq in range(16)]
                Q.add("dve", _tt(c4, in0, in1, ALU.add), reads=allsv, writes=allsc)
                for h in range(8):
                    Q.add("dve", lambda e, h=h: e.max(out=fv[:, h, 0:8], in_=cand[:, h * 256:(h + 1) * 256]),
                          reads=[("sc", h // 2)], writes=[("fv0", h)])
                for h in range(8):
                    Q.add("dve", lambda e, h=h: e.max_index(out=fi[:, h, 0:8], in_max=fv[:, h, 0:8],
                                                            in_values=cand[:, h * 256:(h + 1) * 256]),
                          reads=[("sc", h // 2), ("fv0", h)], writes=[("fi0", h)])
                for h in range(8):
                    Q.add("dve", lambda e, h=h: e.match_replace(out=sc2[:, h * 256:(h + 1) * 256],
                                                                in_to_replace=fv[:, h, 0:8],
                                                                in_values=cand[:, h * 256:(h + 1) * 256],
                                                                imm_value=NEG),
                          reads=[("sc", h // 2), ("fv0", h)], writes=[("sc2", 2 * h), ("sc2", 2 * h + 1)])
                for h in range(8):
                    Q.add("dve", lambda e, h=h: e.max(out=fv[:, h, 8:16], in_=sc2[:, h * 256:(h + 1) * 256]),
                          reads=[("sc2", 2 * h), ("sc2", 2 * h + 1)], writes=[("fv1", h)])
                for h in range(8):
                    Q.add("dve", lambda e, h=h: e.max_index(out=fi[:, h, 8:16], in_max=fv[:, h, 8:16],
                                                            in_values=sc2[:, h * 256:(h + 1) * 256]),
                          reads=[("sc2", 2 * h), ("sc2", 2 * h + 1), ("fv1", h)], writes=[("fi1", h)])
                allfv = [("fv0", h) for h in range(8)] + [("fv1", h) for h in range(8)]
                allfi = [("fi0", h) for h in range(8)] + [("fi1", h) for h in range(8)]
                Q.add("dve", _tt(fsh[:], fv[:], fv[:, :, 0:1].broadcast_to([128, 8, 16]), ALU.subtract), reads=allfv,
                      writes=["fsh"])
                Q.add("act", _act(ex[:], fsh[:], AF.Exp), reads=["fsh"], writes=["ex"])
                Q.add("dve", lambda e: e.tensor_reduce(out=ssum[:], in_=ex[:], axis=AX.X, op=ALU.add), reads=["ex"],
                      writes=["ssum"])
                Q.add("dve", lambda e: e.reciprocal(out=ssum[:], in_=ssum[:]), reads=["ssum"], writes=["ssum"])
                gview = sel3[:, tt, 256:384].rearrange("p (h k) -> p h k", h=8)
                Q.add("dve", _tt(gview, ex[:], ssum[:].unsqueeze(2).broadcast_to([128, 8, 16]), ALU.mult),
                      reads=["ex", "ssum"], writes=[("sel_g", tt)])
                Q.add("dve", lambda e: e.tensor_single_scalar(out=au[:], in_=fi[:], scalar=4,
                                                              op=ALU.logical_shift_right), reads=allfi, writes=["au"])
                Q.add("dve", lambda e: e.tensor_single_scalar(out=bu[:], in_=fi[:], scalar=15, op=ALU.bitwise_and),
                      reads=allfi, writes=["bu"])
                Q.add("dve", _cp(af[:], au[:]), reads=["au"], writes=["af"])
                Q.add("dve", _cp(bf[:], bu[:]), reads=["bu"], writes=["bf"])
                sif4 = sif[:].rearrange("p (h two) k -> p h two k", two=2)
                eq4 = eq[:].rearrange("p (h k a) -> p h k a", h=8, k=16)
                io4 = iota_f[:, 0:16].unsqueeze(1).unsqueeze(1).broadcast_to([128, 8, 16, 16])
                for which, (srcf, half, off) in enumerate(((af, 0, 0), (bf, 1, 128))):
                    Q.add("dve", _tt(eq4, srcf[:].unsqueeze(3).broadcast_to([128, 8, 16, 16]), io4, ALU.is_equal),
                          reads=["af", "bf", "cst"], writes=["eq"])
                    Q.add("dve", _tt(eq4, eq4, sif4[:, :, half, :].unsqueeze(2).broadcast_to([128, 8, 16, 16]),
                                     ALU.mult), reads=["eq", "sif"], writes=["eq"])
                    Q.add("dve", lambda e, off=off, tt=tt: e.tensor_reduce(
                        out=sel3[:, tt, off:off + 128], in_=eq[:].rearrange("p (s a) -> p s a", a=16), axis=AX.X,
                        op=ALU.add), reads=["eq"], writes=[("sel_i%d" % which, tt)])
                selk = [("sel_g", tt), ("sel_i0", tt), ("sel_i1", tt)]
                if dbg:
                    Q.add("act", _dma(d_sel[n, tt], sel3[:, tt, :]), reads=selk, writes=["d_sel"], dma_sem=("dbg", 1))
                Q = QX[tt]
                for q3 in range(3):
                    Q.add("pe", _tr(pb[7][:, q3 * 128:(q3 + 1) * 128], sel3[:, tt, q3 * 128:(q3 + 1) * 128], ident_f),
                          reads=selk + ["cst"], psum=[7])
                Q.add("act", _act(selT[:, tt, :], pb[7][:, 0:384], AF.Copy), psum=[7], writes=[("selT", tt)])
                Q.add("act", _act(negj[:, tt, :], pb[7][:, 128:256], AF.Copy, scale=-1.0), psum=[7],
                      writes=[("negj", tt)])
            return QT, QX

        g_bank = {"n": 0}

        def gbuild(n):
            if 2 <= n + 2 < n_chunks:
                xi_ = (n + 2) % 3
                S.add("sp", _dma(xT[xi_][:], xd[n + 2]), writes=[("x%d" % xi_, kc) for kc in range(8)],
                      dma_sem=("x", xi_))
            th = []
            for tt in range(2):
                for t4 in range(32):
                    th.append(lambda tt=tt, t4=t4: gb_group(n, tt, t4))
            return th

        def gb_group(n, tt, t4):
            if True:
                if True:
                    gb = g_bank["n"] % 4
                    g_bank["n"] += 1
                    for tq in range(4):
                        tok = t4 * 4 + tq
                        slot = tok % NOH
                        S.add("dve", _ts(oh0[:, slot, :], iota_b[:], selT[:, tt, tok:tok + 1], ALU.is_equal,
                                         selT[:, tt, 256 + tok:256 + tok + 1], ALU.mult),
                              reads=[("selT", tt), "iota_b"], writes=[("oh0", slot)])
                        if tok % 3 == 2:
                            b4 = tok % 4
                            S.add("act", _act(ge[:, b4, 0:128], iota_b[:], AF.Abs, bias=negj[:, tt, tok:tok + 1]),
                                  reads=[("negj", tt), "iota_b"], writes=[("ge", b4)])
                            S.add("act", _act(oh1[:, slot, :], ge[:, b4, 0:128], AF.Relu, scale=-1.0, bias=1.0),
                                  reads=[("ge", b4)], writes=[("oh1", slot)])
                        else:
                            S.add("dve", _ts(oh1[:, slot, :], iota_b[:], selT[:, tt, 128 + tok:128 + tok + 1],
                                             ALU.is_equal),
                                  reads=[("selT", tt), "iota_b"], writes=[("oh1", slot)])
                        S.add("pe", _mm(pb[gb][:, tq * 128:(tq + 1) * 128], oh0[:, slot, :], oh1[:, slot, :]),
                              reads=[("oh0", slot), ("oh1", slot)], psum=[gb])
                    gt0 = tt * 128 + t4 * 4
                    S.add("act", _act(G[:, gt0:gt0 + 4, :], pb[gb][:].rearrange("p (t j) -> p t j", t=4), AF.Copy),
                          psum=[gb], writes=["G"], after=UNI_KEYS if (tt == 0 and t4 == 0) else ())


        def jloop(n, tqs):
            par = n % 2
            npar = 1 - par
            first = (n % 8 == 0)
            X = xT[n % 3]
            xk = "x%d" % (n % 3)
            xn2T = xn2[par]
            xn2k = "xn2T%d" % par
            def peer_A(j):
                wsl, wk = ring_next(B_U + j)
                w3 = wsl.rearrange("p (k m) -> p k m", k=8)
                ps_, pk = mm_slot((4, 5))
                for kc in range(8):
                    S.add("pe", _mm(ps_, w3[:, kc, :], xn2T[:, kc, :], start=(kc == 0), stop=(kc == 7)),
                          reads=[wk, (xn2k, kc)], psum=[pk])
                b4 = j % 4
                S.add("act", _act(ge[:, b4, :], ps_, AF.Gelu), psum=[pk], writes=[("ge", b4)])
                S.add("pool", _tt(wt[:, b4, :], ge[:, b4, :], G[:, :, j], ALU.mult), reads=[("ge", b4), "G"],
                      writes=[("wt", b4)])

            def peer_O(j):
                wsl, wk = ring_next(B_V + j)
                b4 = j % 4
                for m in range(8):
                    S.add("pe", _mm(pb[m // 2][:, (m % 2) * 256:(m % 2 + 1) * 256], wsl[:, m * 128:(m + 1) * 128],
                                    wt[:, b4, :], start=(j == 0 and m % 2 == 0), stop=(j == 127), sgc=True),
                          reads=[wk, ("wt", b4)], psum=[m // 2])

            LAG = 3
            for j in range(128 + LAG):
                if j < 128:
                    peer_A(j)
                if j >= LAG:
                    peer_O(j - LAG)
                if tqs is not None:
                    qt_, qx_ = tqs
                    if j < 56:
                        qt_[0].replay(S, 3)
                    elif j < 64:
                        qt_[0].replay(S)
                        qt_[1].replay(S, 3)
                    elif j < 124:
                        qt_[1].replay(S, 3)
                        if j == 90:
                            qx_[0].replay(S)
                    else:
                        qt_[1].replay(S)
                        if j == 128 + LAG - 1:
                            qx_[1].replay(S)
            for m in range(8):
                S.add("dve", _tt(X[:, m, :], pb[m // 2][:, (m % 2) * 256:(m % 2 + 1) * 256], X[:, m, :], ALU.add),
                      psum=[m // 2], reads=[(xk, m)], writes=[(xk, m)])
            if dbg:
                S.add("act", _dma(d_h3[n], X[:]), reads=[(xk, m) for m in range(8)], writes=["d_h3"],
                      dma_sem=("dbg", 2))


        def final(n):
            par = n % 2
            npar = 1 - par
            first = (n % 8 == 0)
            X = xT[n % 3]
            xk = "x%d" % (n % 3)
            xn2T = xn2[par]
            xn2k = "xn2T%d" % par
            th = rmsnorm_thunks(X, xk, X, xk, C_G + 16, sqs=sq_eq, rs=rs_sif)

            def st():
                S.add("act", _dma(od[n], X[:]), reads=[(xk, m) for m in range(8)], writes=[("od", n % 3)] +
                      [(xk, m) for m in range(8)], dma_sem=("o", n % 3))

            return th + [st]

        if n_chunks > 1:
            S.add("sp", _dma(xT[1][:], xd[1]), writes=[("x1", kc) for kc in range(8)], dma_sem=("x", 1))
        mixer(0)
        for t_ in mixer_tail(0):
            t_()
        qt_, qx_ = topk_q(0)
        for i_ in range(2):
            qt_[i_].replay(S)
            qx_[i_].replay(S)
        pending_fin = []
        for n_ in range(n_chunks):
            tqs = None
            tail = []
            if n_ + 1 < n_chunks:
                mixer(n_ + 1, pending_fin)
                pending_fin = []
                tail = mixer_tail(n_ + 1)
                tqs = topk_q(n_ + 1)
            else:
                for t_ in pending_fin:
                    t_()
                pending_fin = []
            gbt = gbuild(n_)
            while gbt or tail:
                for _ in range(3):
                    if gbt:
                        gbt.pop(0)()
                if tail:
                    tail.pop(0)()
            jloop(n_, tqs)
            pending_fin = final(n_)
        for t_ in pending_fin:
            t_()
        last_out_keys = [("od", 0), ("od", 1), ("od", 2)]
        fin_reads = list(last_out_keys)
        if dbg:
            fin_reads += ["d_h2", "d_sel", "d_h3"]
        S.add("act", lambda e: e.nop(), reads=fin_reads)

        S.finalize()
        sems = {}
        for e in S.ENGS:
            sems[("eng", e)] = es.enter_context(nc.semaphore("s_" + e))
        for i, k in enumerate(S.dma_sems):
            sems[("dma", k)] = es.enter_context(nc.semaphore("d_%d" % i))
        with nc.Block() as block:
            @block.sync
            def _(eng):
                S.emit_engine("sp", eng, sems)

            @block.tensor
            def _(eng):
                S.emit_engine("pe", eng, sems)

            @block.scalar
            def _(eng):
                S.emit_engine("act", eng, sems)

            @block.vector
            def _(eng):
                S.emit_engine("dve", eng, sems)

            @block.gpsimd
            def _(eng):
                S.emit_engine("pool", eng, sems)
    return nc


def _blocks8(w):
    K, N = w.shape
    n = N // 128
    kk = K // 128
    return np.ascontiguousarray(w.reshape(kk, 128, n, 128).transpose(2, 1, 0, 3)).reshape(n, 128, kk * 128)


def prep_weights(inp):
    w_in = np.asarray(inp["w_in"][0], dtype=np.float32)
    b_in = np.asarray(inp["b_in"][0], dtype=np.float32)

    def cols(a):
        q, k, v = a[..., 0:1024], a[..., 1024:1152], a[..., 1152:1280]
        pzc, ga, gp = a[..., 1280:1792], a[..., 1792:2816], a[..., 2816:3840]
        k0, k1, v0, v1 = k[..., :64], k[..., 64:], v[..., :64], v[..., 64:]
        return np.concatenate([q, k0, k0, k1, k1, v0, v0, v1, v1, pzc, ga, gp], axis=-1)

    wcols = cols(w_in)
    bcols = cols(b_in)
    wf = np.zeros((NBLK, 128, 1024), dtype=np.float32)
    wf[B_WIN:B_WIN + 32] = _blocks8(wcols)
    wf[B_AUP:B_AUP + 8] = _blocks8(np.asarray(inp["w_attn_up"][0], dtype=np.float32))
    pu = _blocks8(np.asarray(inp["w_pool_up"][0], dtype=np.float32))
    wf[B_PUP:B_PUP + 4] = pu.reshape(4, 2, 128, 512).transpose(0, 2, 1, 3).reshape(4, 128, 1024)
    wf[B_WO:B_WO + 8] = _blocks8(np.asarray(inp["w_o"][0], dtype=np.float32))
    wf[B_WQ:B_WQ + 16] = _blocks8(np.asarray(inp["w_query"][0], dtype=np.float32))
    u = np.asarray(inp["u_experts"][0], dtype=np.float32)
    wf[B_U:B_U + 128] = u.reshape(128, 128, 8, 128).transpose(1, 3, 2, 0).reshape(128, 128, 1024)
    v = np.asarray(inp["v_experts"][0], dtype=np.float32)
    wf[B_V:B_V + 128] = v.reshape(128, 128, 1024).transpose(1, 0, 2)
    grp = np.asarray(inp["w_pool_grp"][0], dtype=np.float32)
    wf[B_GRP, :, 0:512] = grp.transpose(1, 0, 2).reshape(128, 512)
    sk = np.asarray(inp["sub_keys"][0], dtype=np.float32)
    skt = sk.transpose(3, 0, 1, 2).reshape(128, 2048)
    wf[B_SK] = skt[:, 0:1024]
    wf[B_SK + 1] = skt[:, 1024:2048]

    cst = np.zeros((128, NCST), dtype=np.float32)
    for i, nm in enumerate(("ln_mix_g", "ln_ffn_g", "ln_final_g")):
        gv = np.asarray(inp[nm], dtype=np.float32).reshape(-1)
        cst[:, C_G + 8 * i:C_G + 8 * i + 8] = gv.reshape(8, 128).T
    cst[:, C_B:C_B + 32] = bcols.reshape(32, 128).T
    sinks = np.asarray(inp["attn_sinks"][0], dtype=np.float32)
    for c in range(8):
        cst[0:64, C_SINK + c] = sinks[2 * c]
        cst[64:128, C_SINK + c] = sinks[2 * c + 1]
    cst[:, C_PSC:C_PSC + 4] = np.asarray(inp["pool_scale"][0], dtype=np.float32).reshape(4, 128).T
    cst[:, C_ID:C_ID + 128] = np.eye(128, dtype=np.float32)
    cst[:, C_ONES:C_ONES + 128] = 1.0
    cst[:, C_IOTA:C_IOTA + 128] = np.arange(128, dtype=np.float32)[None, :]
    kk = np.arange(128)[:, None]
    qq = np.arange(128)[None, :]
    MNEG = -30000.0
    prevm = np.where(qq < kk, 0.0, MNEG).astype(np.float32)
    diagm = np.where(qq >= kk, 0.0, MNEG).astype(np.float32)
    cst[:, C_MREST:C_MREST + 512] = np.concatenate([prevm, diagm, prevm, diagm], axis=1)
    cst[:, C_MFIRST:C_MFIRST + 512] = np.concatenate([0 * prevm + MNEG, diagm, prevm, diagm], axis=1)
    cst[:, C_OLO:C_OLO + 64] = 1.0
    cst[:, C_OHI + 64:C_OHI + 128] = 1.0
    for g in range(4):
        w = 2 << g
        cst[:, C_RC + g * 16:C_RC + (g + 1) * 16] = (1.0 / np.minimum(np.arange(1, 17), w)).astype(np.float32)[None]
    return wf, cst


def prep_x(x, core, n_chunks=NCH_FULL):
    xs = np.asarray(x[core * 4:(core + 1) * 4], dtype=np.float32).reshape(4 * 2048, 1024)[: n_chunks * T]
    return np.ascontiguousarray(xs.reshape(n_chunks, T, 8, 128).transpose(0, 3, 2, 1))


def unprep_out(od):
    n = od.shape[0]
    return np.ascontiguousarray(od.transpose(0, 3, 2, 1)).reshape(n * T, 1024)


def kernel(**inputs):
    wf, cst = prep_weights(inputs)
    x = inputs["x"]
    nc = build_nc(NCH_FULL)
    in_maps = [{"xd": prep_x(x, c), "wf": wf, "cst": cst} for c in range(N_CORES)]
    res = run_bass_kernel_spmd(nc, in_maps, core_ids=list(range(N_CORES)))
    out = np.concatenate([unprep_out(np.asarray(r["od"])) for r in res.results], axis=0)
    return out.reshape(32, 2048, 1024).astype(np.float32)
```

```python
import numpy as np
from contextlib import ExitStack
import concourse.bass as bass
import concourse.mybir as mybir
from concourse.bass_utils import run_bass_kernel_spmd

F32 = mybir.dt.float32
BF16 = mybir.dt.bfloat16
U32 = mybir.dt.uint32
AF = mybir.ActivationFunctionType
ALU = mybir.AluOpType
AX = mybir.AxisListType

N_CORES = 8
T = 256
NCH_FULL = 32
EPS = 1e-5
NEG = -1e30

B_WIN, B_AUP, B_PUP, B_WO, B_WQ, B_U, B_V, B_GRP, B_SK = 0, 32, 40, 44, 52, 68, 196, 324, 325
NBLK = 327
C_G, C_B, C_SINK, C_PSC, C_ID, C_ONES, C_IOTA, C_MREST, C_MFIRST, C_OLO, C_OHI, C_RC = (
    0, 24, 56, 64, 68, 196, 324, 452, 964, 1476, 1604, 1732)
NCST = 1796


class _Op:
    __slots__ = ("eng", "emit", "deps", "need_inc", "ms", "dma_sem")

    def __init__(self, eng, emit, dma_sem=None):
        self.eng = eng
        self.emit = emit
        self.deps = []
        self.need_inc = False
        self.ms = None
        self.dma_sem = dma_sem


class Sched:
    ENGS = ("pe", "act", "dve", "pool", "sp")

    def __init__(self):
        self.ops = {e: [] for e in self.ENGS}
        self.last_writer = {}
        self.readers = {}
        self.dma_sems = []
        self.bank_acc = {}

    def add(self, eng, emit, reads=(), writes=(), after=(), dma_sem=None, psum=()):
        op = _Op(eng, emit, dma_sem)
        deps = []
        for b in psum:
            acc = self.bank_acc.setdefault(b, {})
            for e2, op2 in acc.items():
                if e2 != eng:
                    deps.append(op2)
            acc[eng] = op
        for k in reads:
            w = self.last_writer.get(k)
            if w is not None:
                deps.append(w)
        for k in tuple(writes) + tuple(after):
            w = self.last_writer.get(k)
            if w is not None:
                deps.append(w)
            deps.extend(self.readers.get(k, ()))
        seen = set()
        for d in deps:
            if id(d) in seen:
                continue
            seen.add(id(d))
            if d.eng == "pe" and eng == "pe":
                continue
            op.deps.append(d)
            d.need_inc = True
        for k in reads:
            self.readers.setdefault(k, []).append(op)
        for k in writes:
            self.last_writer[k] = op
            self.readers[k] = []
        if dma_sem is not None and dma_sem not in self.dma_sems:
            self.dma_sems.append(dma_sem)
        self.ops[eng].append(op)
        return op

    def finalize(self):
        cnt = {e: 0 for e in self.ENGS}
        dcnt = {}
        for e in self.ENGS:
            for op in self.ops[e]:
                if op.dma_sem is not None:
                    dcnt[op.dma_sem] = dcnt.get(op.dma_sem, 0) + 16
                    op.ms = (("dma", op.dma_sem), dcnt[op.dma_sem])
                elif op.need_inc:
                    cnt[e] += 1
                    op.ms = (("eng", e), cnt[e])
        return cnt

    def emit_engine(self, e, eng, sems):
        waited = {}
        for op in self.ops[e]:
            need = {}
            for d in op.deps:
                k, v = d.ms
                if waited.get(k, 0) >= v:
                    continue
                if need.get(k, 0) < v:
                    need[k] = v
            for k, v in need.items():
                eng.wait_ge(sems[k], v)
                waited[k] = v
            ins = op.emit(eng)
            if op.ms is not None:
                k, v = op.ms
                ins.then_inc(sems[k], 16 if k[0] == "dma" else 1)


class _DQ:
    def __init__(self):
        self.l = []

    def add(self, *a, **k):
        self.l.append((a, k))

    def replay(self, S, n=None):
        m = len(self.l) if n is None else min(n, len(self.l))
        for a, k in self.l[:m]:
            S.add(*a, **k)
        del self.l[:m]


def _mm(out, lhsT, rhs, start=True, stop=True, sgc=False):
    if sgc:
        return lambda e: e.matmul(out=out, lhsT=lhsT, rhs=rhs, start=start, stop=stop, skip_group_check=True)
    return lambda e: e.matmul(out=out, lhsT=lhsT, rhs=rhs, start=start, stop=stop)


def _tr(out, in_, ident):
    return lambda e: e.transpose(out=out, in_=in_, identity=ident)


def _act(out, in_, func, bias=None, scale=None):
    kw = {}
    if bias is not None:
        kw["bias"] = bias
    if scale is not None:
        kw["scale"] = scale
    return lambda e: e.activation(out=out, in_=in_, func=func, **kw)


def _tt(out, in0, in1, op):
    return lambda e: e.tensor_tensor(out=out, in0=in0, in1=in1, op=op)


def _ts(out, in0, s1, op0, s2=None, op1=None):
    if op1 is None:
        return lambda e: e.tensor_scalar(out=out, in0=in0, scalar1=s1, scalar2=None, op0=op0)
    return lambda e: e.tensor_scalar(out=out, in0=in0, scalar1=s1, scalar2=s2, op0=op0, op1=op1)


def _stt(out, in0, scalar, in1, op0, op1):
    return lambda e: e.scalar_tensor_tensor(out=out, in0=in0, scalar=scalar, in1=in1, op0=op0, op1=op1)


def _cp(out, in_):
    return lambda e: e.tensor_copy(out=out, in_=in_)


def _dma(out, in_):
    return lambda e: e.dma_start(out=out, in_=in_)


class _Stop(Exception):
    pass


def build_nc(n_chunks=NCH_FULL, dbg=False, stage=99):
    nc = bass.Bass("TRN2", target_bir_lowering=False)
    xd = nc.dram_tensor("xd", [n_chunks, 128, 8, T], F32, kind="ExternalInput").ap()
    wf = nc.dram_tensor("wf", [NBLK, 128, 1024], F32, kind="ExternalInput").ap()
    cstd = nc.dram_tensor("cst", [128, NCST], F32, kind="ExternalInput").ap()
    wb = nc.dram_tensor("wb", [NBLK, 128, 1024], BF16, kind="Internal").ap()
    od = nc.dram_tensor("od", [n_chunks, 128, 8, T], F32, kind="ExternalOutput").ap()
    if dbg:
        d_h2 = nc.dram_tensor("d_h2", [n_chunks, 128, 8, T], F32, kind="ExternalOutput").ap()
        d_sel = nc.dram_tensor("d_sel", [n_chunks, 2, 128, 384], F32, kind="ExternalOutput").ap()
        d_h3 = nc.dram_tensor("d_h3", [n_chunks, 128, 8, T], F32, kind="ExternalOutput").ap()

    S = Sched()
    es = ExitStack()
    with es:
        def sb(name, shape, dt):
            return es.enter_context(nc.sbuf_tensor(name, shape, dt))

        NB = 7
        xT = [sb("xT%d" % i_, [128, 8, T], F32) for i_ in range(3)]
        uni = sb("uni", [128, 16384], F32)
        unib = uni[:].bitcast(BF16)

        def uf(off, n):
            return uni[:, off // 4: off // 4 + n]

        def ub(off, n):
            return unib[:, off // 2: off // 2 + n]

        sga = uf(0, 2048).rearrange("p (m t) -> p m t", m=8)
        sgp = uf(8192, 2048).rearrange("p (m t) -> p m t", m=8)
        tmpA = uf(16384, 2048).rearrange("p (m t) -> p m t", m=8)
        sq = uf(24576, 2048).rearrange("p (m t) -> p m t", m=8)
        xnT = ub(32768, 2048).rearrange("p (m t) -> p m t", m=8)
        mergedT = ub(36864, 2048).rearrange("p (m t) -> p m t", m=8)
        qT = ub(40960, 2048).rearrange("p (m t) -> p m t", m=8)
        attnT = ub(45056, 2048).rearrange("p (m t) -> p m t", m=8)
        Ebuf = ub(49152, 2048).rearrange("p (m t) -> p m t", m=4)
        PTb = ub(53248, 2048).rearrange("p (m t) -> p m t", m=4)
        rd = uf(57344, 512).rearrange("p (m t) -> p m t", m=2)
        tmpB = uf(59392, 512).rearrange("p (m t) -> p m t", m=2)
        G = unib.rearrange("p (t j) -> p t j", j=128)
        NPS = 4
        pst_f = [uf(i_ * 8192, 2048) for i_ in range(NPS)]
        pst_b = [ub(32768 + i_ * 4096, 2048) for i_ in range(NPS)]
        PSTK = ["pstf%d" % i_ for i_ in range(NPS)] + ["pstb%d" % i_ for i_ in range(NPS)]
        UNI_KEYS = ["sga", "sgp", "tmpA", "sq", "xnT", "mergedT", "qT", "attnT", "E0", "E1", "E2", "E3",
                    "PT0", "PT1", "PT2", "PT3", "rd0", "rd1", "tmpB0", "tmpB1"] + PSTK
        for m in range(8):
            UNI_KEYS += [("sga", m), ("sgp", m), ("tmpA", m), ("sq", m), ("xnT", m), ("mergedT", m), ("qT", m),
                         ("attnT", m)]
        GK = ["G"] + PSTK

        xn2 = [sb("xn2Ta", [128, 8, T], BF16), sb("xn2Tb", [128, 8, T], BF16)]
        rstd = sb("rstd", [128, T], F32)
        kT = sb("kT", [128, 2, 2, 384], BF16)
        vT = sb("vT", [128, 2, T], BF16)
        vlo = sb("vlo", [128, 2, 2, 3, 128], BF16)
        vhi = sb("vhi", [128, 2, 2, 3, 128], BF16)
        pz = sb("pz", [128, 2, 4, 272], F32)
        ptmp = sb("ptmp", [128, 4, 272], F32)
        pfix = sb("pfix", [128, 16], F32)
        pooledT = sb("pooledT", [128, 4, T], BF16)
        ypT = sb("ypT", [128, 4, T], BF16)
        qpT = sb("qpT", [128, 16, T], BF16)
        sc = sb("sc", [128, 2048], F32)
        sc2 = sb("sc2", [128, 2048], F32)
        sv = sb("sv", [128, 16, 16], F32)
        si = sb("si", [128, 16, 16], U32)
        sif = sb("sif", [128, 16, 16], F32)
        fv = sb("fv", [128, 8, 16], F32)
        fi = sb("fi", [128, 8, 16], U32)
        fsh = sb("fsh", [128, 8, 16], F32)
        ex = sb("ex", [128, 8, 16], F32)
        ssum = sb("ssum", [128, 8], F32)
        sel3 = sb("sel3", [128, 2, 384], F32)
        au = sb("au", [128, 8, 16], U32)
        bu = sb("bu", [128, 8, 16], U32)
        af = sb("af", [128, 8, 16], F32)
        bf = sb("bf", [128, 8, 16], F32)
        eq = sb("eq", [128, 2048], F32)
        selT = sb("selT", [128, 2, 384], F32)
        NOH = 8
        oh0 = sb("oh0", [128, NOH, 128], BF16)
        oh1 = sb("oh1", [128, NOH, 128], BF16)
        wt = sb("wt", [128, 4, T], BF16)
        ge = sb("ge", [128, 4, T], BF16)
        ring = sb("ring", [128, NB, 1024], BF16)
        cst = sb("cstt", [128, NCST], F32)
        maskb = sb("maskb", [128, 1024], BF16)
        oneslh = sb("oneslh", [128, 256], BF16)
        identb = sb("identb", [128, 128], BF16)
        iota_b = sb("iota_b", [128, 128], BF16)
        wgrp_b = sb("wgrp_b", [128, 512], BF16)
        skT_b = sb("skT_b", [128, 2048], BF16)
        sinkexp = sb("sinkexp", [128, 8], F32)
        bhalf = sb("bhalf", [128, 16], F32)

        pb = [es.enter_context(nc.psum_tensor("pb%d" % i, [128, 512], F32)) for i in range(8)]

        def PK(bank, half=None):
            if half is None:
                return [("ps", bank, 0), ("ps", bank, 1)]
            return [("ps", bank, half)]

        ident_f = cst[:, C_ID:C_ID + 128]
        ones_f = cst[:, C_ONES:C_ONES + 128]
        iota_f = cst[:, C_IOTA:C_IOTA + 128]

        S.add("sp", _dma(cst[:], cstd), writes=["cst"], dma_sem="cst")
        S.add("dve", _cp(maskb[:], cst[:, C_MREST:C_MREST + 1024]), reads=["cst"], writes=["maskb"])
        S.add("dve", _cp(oneslh[:], cst[:, C_OLO:C_OLO + 256]), reads=["cst"], writes=["oneslh"])
        S.add("dve", _cp(identb[:], ident_f), reads=["cst"], writes=["identb"])
        S.add("dve", _cp(iota_b[:], iota_f), reads=["cst"], writes=["iota_b"])
        S.add("act", _act(sinkexp[:], cst[:, C_SINK:C_SINK + 8], AF.Exp), reads=["cst"], writes=["sinkexp"])
        S.add("dve", _ts(bhalf[:], cst[:, C_B + 16:C_B + 32], 0.5, ALU.mult), reads=["cst"], writes=["bhalf"])
        S.add("pool", lambda e: e.memset(kT[:], 0.0), writes=["kTc0", "kTc1"])
        S.add("pool", lambda e: e.memset(vlo[:], 0.0), writes=["vc0", "vc1"])
        S.add("pool", lambda e: e.memset(vhi[:], 0.0), writes=["vc0", "vc1"])
        S.add("pool", lambda e: e.memset(ptmp[:], 0.0), writes=[("ptmp", l_) for l_ in range(4)])
        S.add("pool", lambda e: e.memset(pz[:], 0.0), writes=[("pz", p_, g_) for p_ in range(2) for g_ in range(4)])

        cast_engs = ["act", "dve", "pool"]
        ngrp = (NBLK + 1) // 2
        LA = NPS - 1
        stores = []
        for gi in range(ngrp + LA):
            if gi < ngrp:
                b0 = gi * 2
                nb = min(2, NBLK - b0)
                s_ = gi % NPS
                src = wf[b0:b0 + nb].rearrange("b p n -> p b n")
                dst = wb[b0:b0 + nb].rearrange("b p n -> p b n")
                fview = pst_f[s_][:, 0:nb * 1024].rearrange("p (b n) -> p b n", b=nb)
                bview = pst_b[s_][:, 0:nb * 1024].rearrange("p (b n) -> p b n", b=nb)
                S.add("sp", _dma(fview, src), writes=["pstf%d" % s_], dma_sem=("pl", s_))
                ce = cast_engs[gi % 3]
                if ce == "act":
                    S.add("act", _act(bview, fview, AF.Copy), reads=["pstf%d" % s_], writes=["pstb%d" % s_])
                else:
                    S.add(ce, _cp(bview, fview), reads=["pstf%d" % s_], writes=["pstb%d" % s_])
                stores.append((dst, bview, s_, b0, nb))
            if gi >= LA:
                dst, bview, s_, b0, nb = stores[gi - LA]
                S.add("sp", _dma(dst, bview), reads=["pstb%d" % s_], writes=[("scr", b0 + i) for i in range(nb)],
                      dma_sem=("pst", s_))
        S.add("sp", _dma(wgrp_b[:], wb[B_GRP, :, 0:512]), reads=[("scr", B_GRP)], writes=["wgrp_b"], dma_sem="rw0")
        S.add("sp", _dma(skT_b[:].rearrange("p (b n) -> p b n", b=2), wb[B_SK:B_SK + 2].rearrange("b p n -> p b n")),
              reads=[("scr", B_SK), ("scr", B_SK + 1)], writes=["skT_b"], dma_sem="rw1")

        ring_state = {"n": 0}

        def ring_next(blk):
            slot = ring_state["n"] % NB
            ring_state["n"] += 1
            key = ("ring", slot)
            S.add("sp", _dma(ring[:, slot, :], wb[blk]), reads=[("scr", blk)], writes=[key], dma_sem=("w", slot))
            return ring[:, slot, :], key

        mm_state = {}

        def mm_slot(banks=(4, 5)):
            i = mm_state.get(banks, 0)
            mm_state[banks] = i + 1
            bank = banks[i % len(banks)]
            return pb[bank][:, 0:256], bank

        sq_uni = (sq, lambda kc: [("sq", kc)], GK, ())
        sq_sc2 = (sc2[:].rearrange("p (m t) -> p m t", m=8), lambda kc: [("sc2", 2 * kc), ("sc2", 2 * kc + 1)], (), ())
        sq_eq = (eq[:].rearrange("p (m t) -> p m t", m=8), lambda kc: [("eqn", kc)], ("eq",), ("eq",))
        rs_std = (rstd, "rstd")
        rs_sif = (sif[:].rearrange("p a b -> p (a b)"), "sif")

        def rmsnorm_thunks(src, kin, dst, kout, gcol, out_after=(), sqs=None, rs=None):
            sqv, sqk, sq_after, sq_xr = sqs if sqs is not None else sq_uni
            rsv, rsk = rs if rs is not None else rs_std
            slot = {}

            def t1():
                for kc in range(8):
                    S.add("act", _act(sqv[:, kc, :], src[:, kc, :], AF.Square), reads=[(kin, kc)], writes=sqk(kc),
                          after=sq_after)

            def t2():
                ps_, pk = mm_slot()
                for kc in range(8):
                    S.add("pe", _mm(ps_, ones_f, sqv[:, kc, :], start=(kc == 0), stop=(kc == 7)),
                          reads=sqk(kc) + ["cst"] + list(sq_xr), psum=[pk])
                S.add("act", _act(rsv[:], ps_, AF.Sqrt, bias=EPS, scale=1.0 / 1024.0), psum=[pk], writes=[rsk])

            def t3():
                S.add("dve", lambda e: e.reciprocal(out=rsv[:], in_=rsv[:]), reads=[rsk], writes=[rsk])

            def t4():
                for kc in range(8):
                    S.add("dve", _stt(dst[:, kc, :], src[:, kc, :], cst[:, gcol + kc:gcol + kc + 1], rsv[:], ALU.mult,
                                      ALU.mult),
                          reads=[(kin, kc), rsk, "cst"], writes=[(kout, kc)], after=out_after)

            return [t1, t2, t3, t4]

        def rmsnorm(src, kin, dst, kout, gcol, out_after=(), sqs=None, rs=None):
            for t_ in rmsnorm_thunks(src, kin, dst, kout, gcol, out_after, sqs, rs):
                t_()

        S.add("sp", _dma(xT[0][:], xd[0]), writes=[("x0", kc) for kc in range(8)], dma_sem=("x", 0))
        def mixer(n, fin_thunks=()):
            fin_thunks = list(fin_thunks)
            par = n % 2
            npar = 1 - par
            first = (n % 8 == 0)
            X = xT[n % 3]
            xk = "x%d" % (n % 3)
            kTc, kTn = "kTc%d" % par, "kTc%d" % npar
            vc, vn = "vc%d" % par, "vc%d" % npar
            pzc, pzn = "pzc%d" % par, "pzc%d" % npar

            rmsnorm(X, xk, xnT, "xnT", C_G + 0, out_after=GK)

            def inproj_chunk(oc):
                wsl, wk = ring_next(B_WIN + oc)
                w3 = wsl.rearrange("p (k m) -> p k m", k=8)
                ps_, pk = mm_slot()
                for kc in range(8):
                    S.add("pe", _mm(ps_, w3[:, kc, :], xnT[:, kc, :], start=(kc == 0), stop=(kc == 7)),
                          reads=[wk, ("xnT", kc)], psum=[pk])
                bcol = cst[:, C_B + oc:C_B + oc + 1]
                if oc < 8:
                    S.add("act", _act(qT[:, oc, :], ps_, AF.Identity, bias=bcol), psum=[pk], reads=["cst"],
                          writes=[("qT", oc)], after=GK)
                elif oc < 10:
                    kv = oc - 8
                    S.add("act", _act(kT[:, par, kv, 128:384], ps_, AF.Identity, bias=bcol), psum=[pk], reads=["cst"],
                          writes=[kTc])
                    S.add("act", _act(kT[:, npar, kv, 0:128], ps_[:, 128:256], AF.Identity, bias=bcol),
                          psum=[pk], reads=["cst"], writes=[kTn])
                elif oc < 12:
                    kv = oc - 10
                    S.add("act", _act(vT[:, kv, :], ps_, AF.Identity, bias=bcol), psum=[pk], reads=["cst"],
                          writes=[("vT", kv)])
                    p3b = pb[3][:].bitcast(BF16)
                    for blk in range(2):
                        qi = kv * 2 + blk
                        o_ = p3b[:, qi * 256:qi * 256 + 128]
                        S.add("pe", _tr(o_, vT[:, kv, blk * 128:(blk + 1) * 128], identb[:]),
                              reads=[("vT", kv), "identb"], psum=[3])
                        S.add("dve", _cp(vlo[:, par, kv, blk + 1, 0:64], o_[:, 0:64]), psum=[3], writes=[vc])
                        S.add("dve", _cp(vhi[:, par, kv, blk + 1, 64:128], o_[:, 64:128]), psum=[3], writes=[vc])
                        if blk == 1:
                            S.add("dve", _cp(vlo[:, npar, kv, 0, 0:64], o_[:, 0:64]), psum=[3], writes=[vn])
                            S.add("dve", _cp(vhi[:, npar, kv, 0, 64:128], o_[:, 64:128]), psum=[3], writes=[vn])
                elif oc < 16:
                    g = oc - 12
                    if first:
                        S.add("pool", lambda e, g=g: e.memset(pz[:, par, g, 0:16], 0.0), writes=[("pz", par, g)])
                    S.add("act", _act(pz[:, par, g, 16:272], ps_, AF.Identity, bias=bcol), psum=[pk], reads=["cst"],
                          writes=[("pz", par, g)])
                    S.add("act", _act(pz[:, npar, g, 0:16], ps_[:, 240:256], AF.Identity, bias=bcol),
                          psum=[pk], reads=["cst"], writes=[("pz", npar, g)])
                elif oc < 24:
                    m = oc - 16
                    S.add("act", _act(sga[:, m, :], ps_, AF.Tanh, bias=bhalf[:, m:m + 1], scale=0.5), psum=[pk],
                          reads=["bhalf"], writes=[("sga", m)], after=GK)
                else:
                    m = oc - 24
                    S.add("act", _act(sgp[:, m, :], ps_, AF.Tanh, bias=bhalf[:, 8 + m:9 + m], scale=0.5), psum=[pk],
                          reads=["bhalf"], writes=[("sgp", m)], after=GK)

            for oc_ in range(12):
                inproj_chunk(oc_)
                if fin_thunks and oc_ % 2 == 0:
                    fin_thunks.pop(0)()
            while fin_thunks:
                fin_thunks.pop(0)()
            f_pool = [(lambda oc_=oc_: inproj_chunk(oc_)) for oc_ in range(12, 16)]

            def pool_group(g):
                w = 2 << g
                p_ = pz[:, par, g, :]
                cur = p_
                curk = [("pz", par, g)]
                sh = 1
                lvl = 0
                while sh < w:
                    dst = ptmp[:, lvl, :]
                    S.add("pool", _tt(dst[:, sh:272], cur[:, sh:272], cur[:, 0:272 - sh], ALU.add),
                          reads=curk, writes=[("ptmp", lvl)])
                    cur = dst
                    curk = [("ptmp", lvl)]
                    sh *= 2
                    lvl += 1
                S.add("dve", _stt(pooledT[:, g, :], cur[:, 16:272], 1.0 / w, p_[:, 16:272], ALU.mult, ALU.subtract),
                      reads=curk + [("pz", par, g)], writes=[("pooledT", g)])
                if first:
                    S.add("dve", _tt(pfix[:], cur[:, 16:32], cst[:, C_RC + g * 16:C_RC + (g + 1) * 16],
                                     ALU.mult), reads=curk + ["cst"], writes=["pfix"])
                    S.add("dve", _tt(pooledT[:, g, 0:16], pfix[:], p_[:, 16:32], ALU.subtract),
                          reads=["pfix", ("pz", par, g)], writes=[("pooledT", g)])
                ps_, pk = mm_slot()
                S.add("pe", _mm(ps_, wgrp_b[:, g * 128:(g + 1) * 128], pooledT[:, g, :]),
                      reads=["wgrp_b", ("pooledT", g)], psum=[pk])
                S.add("act", _act(ypT[:, g, :], ps_, AF.Identity, scale=cst[:, C_PSC + g:C_PSC + g + 1]),
                      psum=[pk], reads=["cst"], writes=[("ypT", g)])

            f_pool += [(lambda g_=g_: pool_group(g_)) for g_ in range(4)]
            f_gate = [(lambda oc_=oc_: inproj_chunk(oc_)) for oc_ in range(16, 32)]
            fillers = []
            for i_ in range(16):
                fillers.append(f_gate[i_])
                if i_ < 8:
                    fillers.append(f_pool[i_])

            def run_fillers(k):
                for _ in range(k):
                    if fillers:
                        fillers.pop(0)()

            def att_scores(c):
                kv = c // 4
                for hh in range(2):
                    bank = (c % 2) * 2 + hh
                    pr = slice(hh * 64, hh * 64 + 64)
                    ps_ = pb[bank]
                    rk = [("qT", c), kTc]
                    S.add("pe", _mm(ps_[:, 0:128], kT[pr, par, kv, 0:128], qT[pr, c, 0:128], start=True, stop=False,
                                    sgc=True), reads=rk, psum=[bank])
                    S.add("pe", _mm(ps_[:, 128:384], kT[pr, par, kv, 128:256], qT[pr, c, 0:256], start=False,
                                    stop=False, sgc=True), reads=rk, psum=[bank])
                    S.add("pe", _mm(ps_[:, 384:512], kT[pr, par, kv, 256:384], qT[pr, c, 128:256], start=False,
                                    stop=False, sgc=True), reads=rk, psum=[bank])
                    mk = maskb[:, 512:1024] if first else maskb[:, 0:512]
                    S.add("pe", _mm(ps_[:, 0:512], identb[:], mk, start=False, stop=True, sgc=True),
                          reads=["identb", "maskb"], psum=[bank])
                    eb = (c % 2) * 2 + hh
                    S.add("act", _act(PTb[:, eb, :], ps_[:], AF.Exp, scale=0.125), psum=[bank],
                          writes=["PT%d" % eb], after=GK)

            def att_pv(c):
                kv = c // 4
                ob = 6 + (c % 2)
                o_ps = pb[ob][:, 0:256]
                d_ps = pb[ob][:, 256:512]
                for blk in range(2):
                    seq = []
                    for hh in range(2):
                        eb = (c % 2) * 2 + hh
                        vv = vlo if hh == 0 else vhi
                        on = oneslh[:, hh * 128:(hh + 1) * 128]
                        for kb in range(2):
                            col = (blk * 2 + kb) * 128
                            seq.append((vv[:, par, kv, blk + kb, :], on, PTb[:, eb, col:col + 128], "PT%d" % eb))
                    for i_, (vw, on, rhs, rkey) in enumerate(seq):
                        S.add("pe", _mm(o_ps[:, blk * 128:(blk + 1) * 128], vw, rhs, start=(i_ == 0), stop=(i_ == 3)),
                              reads=[rkey, vc], psum=[ob])
                    for i_, (vw, on, rhs, rkey) in enumerate(seq):
                        S.add("pe", _mm(d_ps[:, blk * 128:(blk + 1) * 128], on, rhs, start=(i_ == 0), stop=(i_ == 3)),
                              reads=[rkey, "oneslh"], psum=[ob])
                r_ = c % 2
                S.add("dve", _ts(rd[:, r_, :], d_ps, sinkexp[:, c:c + 1], ALU.add), psum=[ob], reads=["sinkexp"],
                      writes=["rd%d" % r_], after=GK)
                S.add("dve", lambda e, r_=r_: e.reciprocal(out=rd[:, r_, :], in_=rd[:, r_, :]), reads=["rd%d" % r_],
                      writes=["rd%d" % r_])
                S.add("dve", _tt(attnT[:, c, :], o_ps, rd[:, r_, :], ALU.mult), psum=[ob], reads=["rd%d" % r_],
                      writes=[("attnT", c)], after=GK)

            att_scores(0)
            run_fillers(2)
            for c in range(8):
                if c + 1 < 8:
                    att_scores(c + 1)
                    run_fillers(2)
                att_pv(c)
                run_fillers(1)
            run_fillers(100)

            for m in range(8):
                wsl, wk = ring_next(B_AUP + m)
                w3 = wsl.rearrange("p (k m) -> p k m", k=8)
                ps_, pk = mm_slot()
                for kc in range(8):
                    S.add("pe", _mm(ps_, w3[:, kc, :], attnT[:, kc, :], start=(kc == 0), stop=(kc == 7)),
                          reads=[wk, ("attnT", kc)], psum=[pk])
                S.add("dve", _stt(tmpA[:, m, :], sga[:, m, :], 1.0, ps_, ALU.add, ALU.mult), psum=[pk], reads=[("sga", m)],
                      writes=[("tmpA", m)], after=GK)
            for mb in range(4):
                wsl, wk = ring_next(B_PUP + mb)
                w4 = wsl.rearrange("p (i k m) -> p i k m", i=2, k=4)
                for mi in range(2):
                    m = mb * 2 + mi
                    ps_, pk = mm_slot()
                    for kc in range(4):
                        S.add("pe", _mm(ps_, w4[:, mi, kc, :], ypT[:, kc, :], start=(kc == 0), stop=(kc == 3)),
                              reads=[wk, ("ypT", kc)], psum=[pk])
                    tb = m % 2
                    S.add("dve", _stt(tmpB[:, tb, :], sgp[:, m, :], 1.0, ps_, ALU.add, ALU.mult), psum=[pk], reads=[("sgp", m)],
                          writes=["tmpB%d" % tb], after=GK)
                    S.add("pool", _tt(mergedT[:, m, :], tmpB[:, tb, :], tmpA[:, m, :], ALU.add),
                          reads=["tmpB%d" % tb, ("tmpA", m)], writes=[("mergedT", m)], after=GK)
            for m in range(8):
                wsl, wk = ring_next(B_WO + m)
                w3 = wsl.rearrange("p (k m) -> p k m", k=8)
                ps_, pk = mm_slot()
                for kc in range(8):
                    S.add("pe", _mm(ps_, w3[:, kc, :], mergedT[:, kc, :], start=(kc == 0), stop=(kc == 7)),
                          reads=[wk, ("mergedT", kc)], psum=[pk])
                S.add("dve", _stt(X[:, m, :], ps_, 0.5, X[:, m, :], ALU.mult, ALU.add), psum=[pk], reads=[(xk, m)],
                      writes=[(xk, m)])
            if dbg:
                S.add("act", _dma(d_h2[n], X[:]), reads=[(xk, m) for m in range(8)], writes=["d_h2"],
                      dma_sem=("dbg", 0))

        def mixer_tail(n):
            par = n % 2
            X = xT[n % 3]
            xk = "x%d" % (n % 3)
            th = rmsnorm_thunks(X, xk, xn2[par], "xn2T%d" % par, C_G + 8, sqs=sq_sc2)

            def qchunk(oc):
                wsl, wk = ring_next(B_WQ + oc)
                w3 = wsl.rearrange("p (k m) -> p k m", k=8)
                ps_, pk = mm_slot()
                for kc in range(8):
                    S.add("pe", _mm(ps_, w3[:, kc, :], xn2[par][:, kc, :], start=(kc == 0), stop=(kc == 7)),
                          reads=[wk, ("xn2T%d" % par, kc)], psum=[pk])
                S.add("act", _act(qpT[:, oc, :], ps_, AF.Copy), psum=[pk], writes=[("qpT", oc)])

            th += [(lambda oc_=oc_: qchunk(oc_)) for oc_ in range(16)]
            return th

        def topk_q(n):
            QT = [_DQ(), _DQ()]
            QX = [_DQ(), _DQ()]
            for tt in range(2):
                tsl = slice(tt * 128, (tt + 1) * 128)
                Q = QT[tt]
                for b4_ in range(4):
                    bank = 6 + (b4_ % 2)
                    for i_ in range(4):
                        oc = b4_ * 4 + i_
                        Q.add("pe", _mm(pb[bank][:, i_ * 128:(i_ + 1) * 128], qpT[:, oc, tsl],
                                        skT_b[:, oc * 128:(oc + 1) * 128]),
                              reads=[("qpT", oc), "skT_b"], psum=[bank])
                    Q.add("act", _act(sc[:, b4_ * 512:(b4_ + 1) * 512], pb[bank][:], AF.Copy), psum=[bank],
                          writes=[("sc", b4_)])
                for gq in range(16):
                    Q.add("dve", lambda e, gq=gq: e.max(out=sv[:, gq, 0:8], in_=sc[:, gq * 128:(gq + 1) * 128]),
                          reads=[("sc", gq // 4)], writes=[("sv0", gq)])
                for gq in range(16):
                    Q.add("dve", lambda e, gq=gq: e.max_index(out=si[:, gq, 0:8], in_max=sv[:, gq, 0:8],
                                                              in_values=sc[:, gq * 128:(gq + 1) * 128]),
                          reads=[("sc", gq // 4), ("sv0", gq)], writes=[("si0", gq)])
                for gq in range(16):
                    Q.add("dve", lambda e, gq=gq: e.match_replace(out=sc2[:, gq * 128:(gq + 1) * 128],
                                                                  in_to_replace=sv[:, gq, 0:8],
                                                                  in_values=sc[:, gq * 128:(gq + 1) * 128],
                                                                  imm_value=NEG),
                          reads=[("sc", gq // 4), ("sv0", gq)], writes=[("sc2", gq)])
                for gq in range(16):
                    Q.add("dve", lambda e, gq=gq: e.max(out=sv[:, gq, 8:16], in_=sc2[:, gq * 128:(gq + 1) * 128]),
                          reads=[("sc2", gq)], writes=[("sv1", gq)])
                for gq in range(16):
                    Q.add("dve", lambda e, gq=gq: e.max_index(out=si[:, gq, 8:16], in_max=sv[:, gq, 8:16],
                                                              in_values=sc2[:, gq * 128:(gq + 1) * 128]),
                          reads=[("sc2", gq), ("sv1", gq)], writes=[("si1", gq)])
                allsv = [("sv0", gq) for gq in range(16)] + [("sv1", gq) for gq in range(16)]
                allsi = [("si0", gq) for gq in range(16)] + [("si1", gq) for gq in range(16)]
                Q.add("dve", _cp(sif[:], si[:]), reads=allsi, writes=["sif"])
                cand = sc
                sv4 = sv[:].rearrange("p (h two) k -> p h two k", two=2)
                c4 = cand[:].rearrange("p (h a b) -> p h a b", h=8, a=16)
                in0 = sv4[:, :, 0, :].unsqueeze(3).broadcast_to([128, 8, 16, 16])
                in1 = sv4[:, :, 1, :].unsqueeze(2).broadcast_to([128, 8, 16, 16])
                allsc = [("sc", b) for b in range(4)]
                allsc2 = [("sc2", gq) for gq in range(16)]
                Q.add("dve", _tt(c4, in0, in1, ALU.add), reads=allsv, writes=allsc)
                for h in range(8):
                    Q.add("dve", lambda e, h=h: e.max(out=fv[:, h, 0:8], in_=cand[:, h * 256:(h + 1) * 256]),
                          reads=[("sc", h // 2)], writes=[("fv0", h)])
                for h in range(8):
                    Q.add("dve", lambda e, h=h: e.max_index(out=fi[:, h, 0:8], in_max=fv[:, h, 0:8],
                                                            in_values=cand[:, h * 256:(h + 1) * 256]),
                          reads=[("sc", h // 2), ("fv0", h)], writes=[("fi0", h)])
                for h in range(8):
                    Q.add("dve", lambda e, h=h: e.match_replace(out=sc2[:, h * 256:(h + 1) * 256],
                                                                in_to_replace=fv[:, h, 0:8],
                                                                in_values=cand[:, h * 256:(h + 1) * 256],
                                                                imm_value=NEG),
                          reads=[("sc", h // 2), ("fv0", h)], writes=[("sc2", 2 * h), ("sc2", 2 * h + 1)])
                for h in range(8):
                    Q.add("dve", lambda e, h=h: e.max(out=fv[:, h, 8:16], in_=sc2[:, h * 256:(h + 1) * 256]),
                          reads=[("sc2", 2 * h), ("sc2", 2 * h + 1)], writes=[("fv1", h)])
                for h in range(8):
                    Q.add("dve", lambda e, h=h: e.max_index(out=fi[:, h, 8:16], in_max=fv[:, h, 8:16],
                                                            in_values=sc2[:, h * 256:(h + 1) * 256]),
                          reads=[("sc2", 2 * h), ("sc2", 2 * h + 1), ("fv1", h)], writes=[("fi1", h)])
                allfv = [("fv0", h) for h in range(8)] + [("fv1", h) for h in range(8)]
                allfi = [("fi0", h) for h in range(8)] + [("fi1", h) for h in range(8)]
                Q.add("dve", _tt(fsh[:], fv[:], fv[:, :, 0:1].broadcast_to([128, 8, 16]), ALU.subtract), reads=allfv,
                      writes=["fsh"])
                Q.add("act", _act(ex[:], fsh[:], AF.Exp), reads=["fsh"], writes=["ex"])
                Q.add("dve", lambda e: e.tensor_reduce(out=ssum[:], in_=ex[:], axis=AX.X, op=ALU.add), reads=["ex"],
                      writes=["ssum"])
                Q.add("dve", lambda e: e.reciprocal(out=ssum[:], in_=ssum[:]), reads=["ssum"], writes=["ssum"])
                gview = sel3[:, tt, 256:384].rearrange("p (h k) -> p h k", h=8)
                Q.add("dve", _tt(gview, ex[:], ssum[:].unsqueeze(2).broadcast_to([128, 8, 16]), ALU.mult),
                      reads=["ex", "ssum"], writes=[("sel_g", tt)])
                Q.add("dve", lambda e: e.tensor_single_scalar(out=au[:], in_=fi[:], scalar=4,
                                                              op=ALU.logical_shift_right), reads=allfi, writes=["au"])
                Q.add("dve", lambda e: e.tensor_single_scalar(out=bu[:], in_=fi[:], scalar=15, op=ALU.bitwise_and),
                      reads=allfi, writes=["bu"])
                Q.add("dve", _cp(af[:], au[:]), reads=["au"], writes=["af"])
                Q.add("dve", _cp(bf[:], bu[:]), reads=["bu"], writes=["bf"])
                sif4 = sif[:].rearrange("p (h two) k -> p h two k", two=2)
                eq4 = eq[:].rearrange("p (h k a) -> p h k a", h=8, k=16)
                io4 = iota_f[:, 0:16].unsqueeze(1).unsqueeze(1).broadcast_to([128, 8, 16, 16])
                for which, (srcf, half, off) in enumerate(((af, 0, 0), (bf, 1, 128))):
                    Q.add("dve", _tt(eq4, srcf[:].unsqueeze(3).broadcast_to([128, 8, 16, 16]), io4, ALU.is_equal),
                          reads=["af", "bf", "cst"], writes=["eq"])
                    Q.add("dve", _tt(eq4, eq4, sif4[:, :, half, :].unsqueeze(2).broadcast_to([128, 8, 16, 16]),
                                     ALU.mult), reads=["eq", "sif"], writes=["eq"])
                    Q.add("dve", lambda e, off=off, tt=tt: e.tensor_reduce(
                        out=sel3[:, tt, off:off + 128], in_=eq[:].rearrange("p (s a) -> p s a", a=16), axis=AX.X,
                        op=ALU.add), reads=["eq"], writes=[("sel_i%d" % which, tt)])
                selk = [("sel_g", tt), ("sel_i0", tt), ("sel_i1", tt)]
                if dbg:
                    Q.add("act", _dma(d_sel[n, tt], sel3[:, tt, :]), reads=selk, writes=["d_sel"], dma_sem=("dbg", 1))
                Q = QX[tt]
                for q3 in range(3):
                    Q.add("pe", _tr(pb[7][:, q3 * 128:(q3 + 1) * 128], sel3[:, tt, q3 * 128:(q3 + 1) * 128], ident_f),
                          reads=selk + ["cst"], psum=[7])
                Q.add("act", _act(selT[:, tt, :], pb[7][:, 0:384], AF.Copy), psum=[7], writes=[("selT", tt)])
            return QT, QX

        g_bank = {"n": 0}

        def gbuild(n):
            if 2 <= n + 2 < n_chunks:
                xi_ = (n + 2) % 3
                S.add("sp", _dma(xT[xi_][:], xd[n + 2]), writes=[("x%d" % xi_, kc) for kc in range(8)],
                      dma_sem=("x", xi_))
            th = []
            for tt in range(2):
                for t4 in range(32):
                    th.append(lambda tt=tt, t4=t4: gb_group(n, tt, t4))
            return th

        def gb_group(n, tt, t4):
            if True:
                if True:
                    gb = g_bank["n"] % 4
                    g_bank["n"] += 1
                    for tq in range(4):
                        tok = t4 * 4 + tq
                        slot = tok % NOH
                        S.add("dve", _ts(oh0[:, slot, :], iota_b[:], selT[:, tt, tok:tok + 1], ALU.is_equal,
                                         selT[:, tt, 256 + tok:256 + tok + 1], ALU.mult),
                              reads=[("selT", tt), "iota_b"], writes=[("oh0", slot)])
                        S.add("dve", _ts(oh1[:, slot, :], iota_b[:], selT[:, tt, 128 + tok:128 + tok + 1],
                                         ALU.is_equal),
                              reads=[("selT", tt), "iota_b"], writes=[("oh1", slot)])
                        S.add("pe", _mm(pb[gb][:, tq * 128:(tq + 1) * 128], oh0[:, slot, :], oh1[:, slot, :]),
                              reads=[("oh0", slot), ("oh1", slot)], psum=[gb])
                    gt0 = tt * 128 + t4 * 4
                    S.add("act", _act(G[:, gt0:gt0 + 4, :], pb[gb][:].rearrange("p (t j) -> p t j", t=4), AF.Copy),
                          psum=[gb], writes=["G"], after=UNI_KEYS if (tt == 0 and t4 == 0) else ())


        def jloop(n, tqs):
            par = n % 2
            npar = 1 - par
            first = (n % 8 == 0)
            X = xT[n % 3]
            xk = "x%d" % (n % 3)
            xn2T = xn2[par]
            xn2k = "xn2T%d" % par
            def peer_A(j):
                wsl, wk = ring_next(B_U + j)
                w3 = wsl.rearrange("p (k m) -> p k m", k=8)
                ps_, pk = mm_slot((4, 5))
                for kc in range(8):
                    S.add("pe", _mm(ps_, w3[:, kc, :], xn2T[:, kc, :], start=(kc == 0), stop=(kc == 7)),
                          reads=[wk, (xn2k, kc)], psum=[pk])
                b4 = j % 4
                S.add("act", _act(ge[:, b4, :], ps_, AF.Gelu), psum=[pk], writes=[("ge", b4)])
                S.add("pool", _tt(wt[:, b4, :], ge[:, b4, :], G[:, :, j], ALU.mult), reads=[("ge", b4), "G"],
                      writes=[("wt", b4)])

            def peer_O(j):
                wsl, wk = ring_next(B_V + j)
                b4 = j % 4
                for m in range(8):
                    S.add("pe", _mm(pb[m // 2][:, (m % 2) * 256:(m % 2 + 1) * 256], wsl[:, m * 128:(m + 1) * 128],
                                    wt[:, b4, :], start=(j == 0 and m % 2 == 0), stop=(j == 127), sgc=True),
                          reads=[wk, ("wt", b4)], psum=[m // 2])

            LAG = 3
            for j in range(128 + LAG):
                if j < 128:
                    peer_A(j)
                if j >= LAG:
                    peer_O(j - LAG)
                if tqs is not None:
                    qt_, qx_ = tqs
                    if j < 56:
                        qt_[0].replay(S, 3)
                    elif j < 64:
                        qt_[0].replay(S)
                        qt_[1].replay(S, 3)
                    elif j < 124:
                        qt_[1].replay(S, 3)
                        if j == 90:
                            qx_[0].replay(S)
                    else:
                        qt_[1].replay(S)
                        if j == 128 + LAG - 1:
                            qx_[1].replay(S)
            for m in range(8):
                S.add("dve", _tt(X[:, m, :], pb[m // 2][:, (m % 2) * 256:(m % 2 + 1) * 256], X[:, m, :], ALU.add),
                      psum=[m // 2], reads=[(xk, m)], writes=[(xk, m)])
            if dbg:
                S.add("act", _dma(d_h3[n], X[:]), reads=[(xk, m) for m in range(8)], writes=["d_h3"],
                      dma_sem=("dbg", 2))


        def final(n):
            par = n % 2
            npar = 1 - par
            first = (n % 8 == 0)
            X = xT[n % 3]
            xk = "x%d" % (n % 3)
            xn2T = xn2[par]
            xn2k = "xn2T%d" % par
            th = rmsnorm_thunks(X, xk, X, xk, C_G + 16, sqs=sq_eq, rs=rs_sif)

            def st():
                S.add("act", _dma(od[n], X[:]), reads=[(xk, m) for m in range(8)], writes=[("od", n % 3)] +
                      [(xk, m) for m in range(8)], dma_sem=("o", n % 3))

            return th + [st]

        if n_chunks > 1:
            S.add("sp", _dma(xT[1][:], xd[1]), writes=[("x1", kc) for kc in range(8)], dma_sem=("x", 1))
        mixer(0)
        for t_ in mixer_tail(0):
            t_()
        qt_, qx_ = topk_q(0)
        for i_ in range(2):
            qt_[i_].replay(S)
            qx_[i_].replay(S)
        pending_fin = []
        for n_ in range(n_chunks):
            tqs = None
            tail = []
            if n_ + 1 < n_chunks:
                mixer(n_ + 1, pending_fin)
                pending_fin = []
                tail = mixer_tail(n_ + 1)
                tqs = topk_q(n_ + 1)
            else:
                for t_ in pending_fin:
                    t_()
                pending_fin = []
            gbt = gbuild(n_)
            while gbt or tail:
                for _ in range(3):
                    if gbt:
                        gbt.pop(0)()
                if tail:
                    tail.pop(0)()
            jloop(n_, tqs)
            pending_fin = final(n_)
        for t_ in pending_fin:
            t_()
        last_out_keys = [("od", 0), ("od", 1), ("od", 2)]
        fin_reads = list(last_out_keys)
        if dbg:
            fin_reads += ["d_h2", "d_sel", "d_h3"]
        S.add("act", lambda e: e.nop(), reads=fin_reads)

        S.finalize()
        sems = {}
        for e in S.ENGS:
            sems[("eng", e)] = es.enter_context(nc.semaphore("s_" + e))
        for i, k in enumerate(S.dma_sems):
            sems[("dma", k)] = es.enter_context(nc.semaphore("d_%d" % i))
        with nc.Block() as block:
            @block.sync
            def _(eng):
                S.emit_engine("sp", eng, sems)

            @block.tensor
            def _(eng):
                S.emit_engine("pe", eng, sems)

            @block.scalar
            def _(eng):
                S.emit_engine("act", eng, sems)

            @block.vector
            def _(eng):
                S.emit_engine("dve", eng, sems)

            @block.gpsimd
            def _(eng):
                S.emit_engine("pool", eng, sems)
    return nc


def _blocks8(w):
    K, N = w.shape
    n = N // 128
    kk = K // 128
    return np.ascontiguousarray(w.reshape(kk, 128, n, 128).transpose(2, 1, 0, 3)).reshape(n, 128, kk * 128)


def prep_weights(inp):
    w_in = np.asarray(inp["w_in"][0], dtype=np.float32)
    b_in = np.asarray(inp["b_in"][0], dtype=np.float32)

    def cols(a):
        q, k, v = a[..., 0:1024], a[..., 1024:1152], a[..., 1152:1280]
        pzc, ga, gp = a[..., 1280:1792], a[..., 1792:2816], a[..., 2816:3840]
        k0, k1, v0, v1 = k[..., :64], k[..., 64:], v[..., :64], v[..., 64:]
        return np.concatenate([q, k0, k0, k1, k1, v0, v0, v1, v1, pzc, ga, gp], axis=-1)

    wcols = cols(w_in)
    bcols = cols(b_in)
    wf = np.zeros((NBLK, 128, 1024), dtype=np.float32)
    wf[B_WIN:B_WIN + 32] = _blocks8(wcols)
    wf[B_AUP:B_AUP + 8] = _blocks8(np.asarray(inp["w_attn_up"][0], dtype=np.float32))
    pu = _blocks8(np.asarray(inp["w_pool_up"][0], dtype=np.float32))
    wf[B_PUP:B_PUP + 4] = pu.reshape(4, 2, 128, 512).transpose(0, 2, 1, 3).reshape(4, 128, 1024)
    wf[B_WO:B_WO + 8] = _blocks8(np.asarray(inp["w_o"][0], dtype=np.float32))
    wf[B_WQ:B_WQ + 16] = _blocks8(np.asarray(inp["w_query"][0], dtype=np.float32))
    u = np.asarray(inp["u_experts"][0], dtype=np.float32)
    wf[B_U:B_U + 128] = u.reshape(128, 128, 8, 128).transpose(1, 3, 2, 0).reshape(128, 128, 1024)
    v = np.asarray(inp["v_experts"][0], dtype=np.float32)
    wf[B_V:B_V + 128] = v.reshape(128, 128, 1024).transpose(1, 0, 2)
    grp = np.asarray(inp["w_pool_grp"][0], dtype=np.float32)
    wf[B_GRP, :, 0:512] = grp.transpose(1, 0, 2).reshape(128, 512)
    sk = np.asarray(inp["sub_keys"][0], dtype=np.float32)
    skt = sk.transpose(3, 0, 1, 2).reshape(128, 2048)
    wf[B_SK] = skt[:, 0:1024]
    wf[B_SK + 1] = skt[:, 1024:2048]

    cst = np.zeros((128, NCST), dtype=np.float32)
    for i, nm in enumerate(("ln_mix_g", "ln_ffn_g", "ln_final_g")):
        gv = np.asarray(inp[nm], dtype=np.float32).reshape(-1)
        cst[:, C_G + 8 * i:C_G + 8 * i + 8] = gv.reshape(8, 128).T
    cst[:, C_B:C_B + 32] = bcols.reshape(32, 128).T
    sinks = np.asarray(inp["attn_sinks"][0], dtype=np.float32)
    for c in range(8):
        cst[0:64, C_SINK + c] = sinks[2 * c]
        cst[64:128, C_SINK + c] = sinks[2 * c + 1]
    cst[:, C_PSC:C_PSC + 4] = np.asarray(inp["pool_scale"][0], dtype=np.float32).reshape(4, 128).T
    cst[:, C_ID:C_ID + 128] = np.eye(128, dtype=np.float32)
    cst[:, C_ONES:C_ONES + 128] = 1.0
    cst[:, C_IOTA:C_IOTA + 128] = np.arange(128, dtype=np.float32)[None, :]
    kk = np.arange(128)[:, None]
    qq = np.arange(128)[None, :]
    MNEG = -30000.0
    prevm = np.where(qq < kk, 0.0, MNEG).astype(np.float32)
    diagm = np.where(qq >= kk, 0.0, MNEG).astype(np.float32)
    cst[:, C_MREST:C_MREST + 512] = np.concatenate([prevm, diagm, prevm, diagm], axis=1)
    cst[:, C_MFIRST:C_MFIRST + 512] = np.concatenate([0 * prevm + MNEG, diagm, prevm, diagm], axis=1)
    cst[:, C_OLO:C_OLO + 64] = 1.0
    cst[:, C_OHI + 64:C_OHI + 128] = 1.0
    for g in range(4):
        w = 2 << g
        cst[:, C_RC + g * 16:C_RC + (g + 1) * 16] = (1.0 / np.minimum(np.arange(1, 17), w)).astype(np.float32)[None]
    return wf, cst


def prep_x(x, core, n_chunks=NCH_FULL):
    xs = np.asarray(x[core * 4:(core + 1) * 4], dtype=np.float32).reshape(4 * 2048, 1024)[: n_chunks * T]
    return np.ascontiguousarray(xs.reshape(n_chunks, T, 8, 128).transpose(0, 3, 2, 1))


def unprep_out(od):
    n = od.shape[0]
    return np.ascontiguousarray(od.transpose(0, 3, 2, 1)).reshape(n * T, 1024)


def kernel(**inputs):
    wf, cst = prep_weights(inputs)
    x = inputs["x"]
    nc = build_nc(NCH_FULL)
    in_maps = [{"xd": prep_x(x, c), "wf": wf, "cst": cst} for c in range(N_CORES)]
    res = run_bass_kernel_spmd(nc, in_maps, core_ids=list(range(N_CORES)))
    out = np.concatenate([unprep_out(np.asarray(r["od"])) for r in res.results], axis=0)
    return out.reshape(32, 2048, 1024).astype(np.float32)
```

```python
import numpy as np
from contextlib import ExitStack
import concourse.bass as bass
import concourse.mybir as mybir
from concourse.bass_utils import run_bass_kernel_spmd

F32 = mybir.dt.float32
BF16 = mybir.dt.bfloat16
U32 = mybir.dt.uint32
AF = mybir.ActivationFunctionType
ALU = mybir.AluOpType
AX = mybir.AxisListType

N_CORES = 8
T = 256
NCH_FULL = 32
EPS = 1e-5
NEG = -1e30

B_WIN, B_AUP, B_PUP, B_WO, B_WQ, B_U, B_V, B_GRP, B_SK = 0, 32, 40, 44, 52, 68, 196, 324, 325
NBLK = 327
C_G, C_B, C_SINK, C_PSC, C_ID, C_ONES, C_IOTA, C_MREST, C_MFIRST, C_OLO, C_OHI, C_RC = (
    0, 24, 56, 64, 68, 196, 324, 452, 964, 1476, 1604, 1732)
NCST = 1796


class _Op:
    __slots__ = ("eng", "emit", "deps", "need_inc", "ms", "dma_sem")

    def __init__(self, eng, emit, dma_sem=None):
        self.eng = eng
        self.emit = emit
        self.deps = []
        self.need_inc = False
        self.ms = None
        self.dma_sem = dma_sem


class Sched:
    ENGS = ("pe", "act", "dve", "pool", "sp")

    def __init__(self):
        self.ops = {e: [] for e in self.ENGS}
        self.last_writer = {}
        self.readers = {}
        self.dma_sems = []
        self.bank_acc = {}

    def add(self, eng, emit, reads=(), writes=(), after=(), dma_sem=None, psum=()):
        op = _Op(eng, emit, dma_sem)
        deps = []
        for b in psum:
            acc = self.bank_acc.setdefault(b, {})
            for e2, op2 in acc.items():
                if e2 != eng:
                    deps.append(op2)
            acc[eng] = op
        for k in reads:
            w = self.last_writer.get(k)
            if w is not None:
                deps.append(w)
        for k in tuple(writes) + tuple(after):
            w = self.last_writer.get(k)
            if w is not None:
                deps.append(w)
            deps.extend(self.readers.get(k, ()))
        seen = set()
        for d in deps:
            if id(d) in seen:
                continue
            seen.add(id(d))
            if d.eng == "pe" and eng == "pe":
                continue
            op.deps.append(d)
            d.need_inc = True
        for k in reads:
            self.readers.setdefault(k, []).append(op)
        for k in writes:
            self.last_writer[k] = op
            self.readers[k] = []
        if dma_sem is not None and dma_sem not in self.dma_sems:
            self.dma_sems.append(dma_sem)
        self.ops[eng].append(op)
        return op

    def finalize(self):
        cnt = {e: 0 for e in self.ENGS}
        dcnt = {}
        for e in self.ENGS:
            for op in self.ops[e]:
                if op.dma_sem is not None:
                    dcnt[op.dma_sem] = dcnt.get(op.dma_sem, 0) + 16
                    op.ms = (("dma", op.dma_sem), dcnt[op.dma_sem])
                elif op.need_inc:
                    cnt[e] += 1
                    op.ms = (("eng", e), cnt[e])
        return cnt

    def emit_engine(self, e, eng, sems):
        waited = {}
        for op in self.ops[e]:
            need = {}
            for d in op.deps:
                k, v = d.ms
                if waited.get(k, 0) >= v:
                    continue
                if need.get(k, 0) < v:
                    need[k] = v
            for k, v in need.items():
                eng.wait_ge(sems[k], v)
                waited[k] = v
            ins = op.emit(eng)
            if op.ms is not None:
                k, v = op.ms
                ins.then_inc(sems[k], 16 if k[0] == "dma" else 1)


class _DQ:
    def __init__(self):
        self.l = []

    def add(self, *a, **k):
        self.l.append((a, k))

    def replay(self, S, n=None):
        m = len(self.l) if n is None else min(n, len(self.l))
        for a, k in self.l[:m]:
            S.add(*a, **k)
        del self.l[:m]


def _mm(out, lhsT, rhs, start=True, stop=True, sgc=False):
    if sgc:
        return lambda e: e.matmul(out=out, lhsT=lhsT, rhs=rhs, start=start, stop=stop, skip_group_check=True)
    return lambda e: e.matmul(out=out, lhsT=lhsT, rhs=rhs, start=start, stop=stop)


def _tr(out, in_, ident):
    return lambda e: e.transpose(out=out, in_=in_, identity=ident)


def _act(out, in_, func, bias=None, scale=None):
    kw = {}
    if bias is not None:
        kw["bias"] = bias
    if scale is not None:
        kw["scale"] = scale
    return lambda e: e.activation(out=out, in_=in_, func=func, **kw)


def _tt(out, in0, in1, op):
    return lambda e: e.tensor_tensor(out=out, in0=in0, in1=in1, op=op)


def _ts(out, in0, s1, op0, s2=None, op1=None):
    if op1 is None:
        return lambda e: e.tensor_scalar(out=out, in0=in0, scalar1=s1, scalar2=None, op0=op0)
    return lambda e: e.tensor_scalar(out=out, in0=in0, scalar1=s1, scalar2=s2, op0=op0, op1=op1)


def _stt(out, in0, scalar, in1, op0, op1):
    return lambda e: e.scalar_tensor_tensor(out=out, in0=in0, scalar=scalar, in1=in1, op0=op0, op1=op1)


def _cp(out, in_):
    return lambda e: e.tensor_copy(out=out, in_=in_)


def _dma(out, in_):
    return lambda e: e.dma_start(out=out, in_=in_)


class _Stop(Exception):
    pass


def build_nc(n_chunks=NCH_FULL, dbg=False, stage=99):
    nc = bass.Bass("TRN2", target_bir_lowering=False)
    xd = nc.dram_tensor("xd", [n_chunks, 128, 8, T], F32, kind="ExternalInput").ap()
    wf = nc.dram_tensor("wf", [NBLK, 128, 1024], F32, kind="ExternalInput").ap()
    cstd = nc.dram_tensor("cst", [128, NCST], F32, kind="ExternalInput").ap()
    wb = nc.dram_tensor("wb", [NBLK, 128, 1024], BF16, kind="Internal").ap()
    od = nc.dram_tensor("od", [n_chunks, 128, 8, T], F32, kind="ExternalOutput").ap()
    if dbg:
        d_h2 = nc.dram_tensor("d_h2", [n_chunks, 128, 8, T], F32, kind="ExternalOutput").ap()
        d_sel = nc.dram_tensor("d_sel", [n_chunks, 2, 128, 384], F32, kind="ExternalOutput").ap()
        d_h3 = nc.dram_tensor("d_h3", [n_chunks, 128, 8, T], F32, kind="ExternalOutput").ap()

    S = Sched()
    es = ExitStack()
    with es:
        def sb(name, shape, dt):
            return es.enter_context(nc.sbuf_tensor(name, shape, dt))

        NB = 7
        xT = [sb("xT%d" % i_, [128, 8, T], F32) for i_ in range(3)]
        uni = sb("uni", [128, 16384], F32)
        unib = uni[:].bitcast(BF16)

        def uf(off, n):
            return uni[:, off // 4: off // 4 + n]

        def ub(off, n):
            return unib[:, off // 2: off // 2 + n]

        sga = uf(0, 2048).rearrange("p (m t) -> p m t", m=8)
        sgp = uf(8192, 2048).rearrange("p (m t) -> p m t", m=8)
        tmpA = uf(16384, 2048).rearrange("p (m t) -> p m t", m=8)
        sq = uf(24576, 2048).rearrange("p (m t) -> p m t", m=8)
        xnT = ub(32768, 2048).rearrange("p (m t) -> p m t", m=8)
        mergedT = ub(36864, 2048).rearrange("p (m t) -> p m t", m=8)
        qT = ub(40960, 2048).rearrange("p (m t) -> p m t", m=8)
        attnT = ub(45056, 2048).rearrange("p (m t) -> p m t", m=8)
        Ebuf = ub(49152, 2048).rearrange("p (m t) -> p m t", m=4)
        PTb = ub(53248, 2048).rearrange("p (m t) -> p m t", m=4)
        rd = uf(57344, 512).rearrange("p (m t) -> p m t", m=2)
        tmpB = uf(59392, 512).rearrange("p (m t) -> p m t", m=2)
        G = unib.rearrange("p (t j) -> p t j", j=128)
        NPS = 4
        pst_f = [uf(i_ * 8192, 2048) for i_ in range(NPS)]
        pst_b = [ub(32768 + i_ * 4096, 2048) for i_ in range(NPS)]
        PSTK = ["pstf%d" % i_ for i_ in range(NPS)] + ["pstb%d" % i_ for i_ in range(NPS)]
        UNI_KEYS = ["sga", "sgp", "tmpA", "sq", "xnT", "mergedT", "qT", "attnT", "E0", "E1", "E2", "E3",
                    "PT0", "PT1", "PT2", "PT3", "rd0", "rd1", "tmpB0", "tmpB1"] + PSTK
        for m in range(8):
            UNI_KEYS += [("sga", m), ("sgp", m), ("tmpA", m), ("sq", m), ("xnT", m), ("mergedT", m), ("qT", m),
                         ("attnT", m)]
        GK = ["G"] + PSTK

        xn2 = [sb("xn2Ta", [128, 8, T], BF16), sb("xn2Tb", [128, 8, T], BF16)]
        rstd = sb("rstd", [128, T], F32)
        kT = sb("kT", [128, 2, 2, 384], BF16)
        vT = sb("vT", [128, 2, T], BF16)
        vlo = sb("vlo", [128, 2, 2, 3, 128], BF16)
        vhi = sb("vhi", [128, 2, 2, 3, 128], BF16)
        pz = sb("pz", [128, 2, 4, 272], F32)
        ptmp = sb("ptmp", [128, 4, 272], F32)
        pfix = sb("pfix", [128, 16], F32)
        pooledT = sb("pooledT", [128, 4, T], BF16)
        ypT = sb("ypT", [128, 4, T], BF16)
        qpT = sb("qpT", [128, 16, T], BF16)
        sc = sb("sc", [128, 2048], F32)
        sc2 = sb("sc2", [128, 2048], F32)
        sv = sb("sv", [128, 16, 16], F32)
        si = sb("si", [128, 16, 16], U32)
        sif = sb("sif", [128, 16, 16], F32)
        fv = sb("fv", [128, 8, 16], F32)
        fi = sb("fi", [128, 8, 16], U32)
        fsh = sb("fsh", [128, 8, 16], F32)
        ex = sb("ex", [128, 8, 16], F32)
        ssum = sb("ssum", [128, 8], F32)
        sel3 = sb("sel3", [128, 2, 384], F32)
        au = sb("au", [128, 8, 16], U32)
        bu = sb("bu", [128, 8, 16], U32)
        af = sb("af", [128, 8, 16], F32)
        bf = sb("bf", [128, 8, 16], F32)
        eq = sb("eq", [128, 2048], F32)
        selT = sb("selT", [128, 2, 384], F32)
        NOH = 8
        oh0 = sb("oh0", [128, NOH, 128], BF16)
        oh1 = sb("oh1", [128, NOH, 128], BF16)
        NWB = 5
        wt = sb("wt", [128, NWB, T], BF16)
        ge = sb("ge", [128, NWB, T], BF16)
        ring = sb("ring", [128, NB, 1024], BF16)
        cst = sb("cstt", [128, NCST], F32)
        maskb = sb("maskb", [128, 1024], BF16)
        oneslh = sb("oneslh", [128, 256], BF16)
        identb = sb("identb", [128, 128], BF16)
        iota_b = sb("iota_b", [128, 128], BF16)
        wgrp_b = sb("wgrp_b", [128, 512], BF16)
        skT_b = sb("skT_b", [128, 2048], BF16)
        sinkexp = sb("sinkexp", [128, 8], F32)
        bhalf = sb("bhalf", [128, 16], F32)

        pb = [es.enter_context(nc.psum_tensor("pb%d" % i, [128, 512], F32)) for i in range(8)]

        def PK(bank, half=None):
            if half is None:
                return [("ps", bank, 0), ("ps", bank, 1)]
            return [("ps", bank, half)]

        ident_f = cst[:, C_ID:C_ID + 128]
        ones_f = cst[:, C_ONES:C_ONES + 128]
        iota_f = cst[:, C_IOTA:C_IOTA + 128]

        S.add("sp", _dma(cst[:], cstd), writes=["cst"], dma_sem="cst")
        S.add("dve", _cp(maskb[:], cst[:, C_MREST:C_MREST + 1024]), reads=["cst"], writes=["maskb"])
        S.add("dve", _cp(oneslh[:], cst[:, C_OLO:C_OLO + 256]), reads=["cst"], writes=["oneslh"])
        S.add("dve", _cp(identb[:], ident_f), reads=["cst"], writes=["identb"])
        S.add("dve", _cp(iota_b[:], iota_f), reads=["cst"], writes=["iota_b"])
        S.add("act", _act(sinkexp[:], cst[:, C_SINK:C_SINK + 8], AF.Exp), reads=["cst"], writes=["sinkexp"])
        S.add("dve", _ts(bhalf[:], cst[:, C_B + 16:C_B + 32], 0.5, ALU.mult), reads=["cst"], writes=["bhalf"])
        S.add("pool", lambda e: e.memset(kT[:], 0.0), writes=["kTc0", "kTc1"])
        S.add("pool", lambda e: e.memset(vlo[:], 0.0), writes=["vc0", "vc1"])
        S.add("pool", lambda e: e.memset(vhi[:], 0.0), writes=["vc0", "vc1"])
        S.add("pool", lambda e: e.memset(ptmp[:], 0.0), writes=[("ptmp", l_) for l_ in range(4)])
        S.add("pool", lambda e: e.memset(pz[:], 0.0), writes=[("pz", p_, g_) for p_ in range(2) for g_ in range(4)])

        cast_engs = ["act", "dve", "pool"]
        ngrp = (NBLK + 1) // 2
        LA = NPS - 1
        stores = []
        for gi in range(ngrp + LA):
            if gi < ngrp:
                b0 = gi * 2
                nb = min(2, NBLK - b0)
                s_ = gi % NPS
                src = wf[b0:b0 + nb].rearrange("b p n -> p b n")
                dst = wb[b0:b0 + nb].rearrange("b p n -> p b n")
                fview = pst_f[s_][:, 0:nb * 1024].rearrange("p (b n) -> p b n", b=nb)
                bview = pst_b[s_][:, 0:nb * 1024].rearrange("p (b n) -> p b n", b=nb)
                S.add("sp", _dma(fview, src), writes=["pstf%d" % s_], dma_sem=("pl", s_))
                ce = cast_engs[gi % 3]
                if ce == "act":
                    S.add("act", _act(bview, fview, AF.Copy), reads=["pstf%d" % s_], writes=["pstb%d" % s_])
                else:
                    S.add(ce, _cp(bview, fview), reads=["pstf%d" % s_], writes=["pstb%d" % s_])
                stores.append((dst, bview, s_, b0, nb))
            if gi >= LA:
                dst, bview, s_, b0, nb = stores[gi - LA]
                S.add("sp", _dma(dst, bview), reads=["pstb%d" % s_], writes=[("scr", b0 + i) for i in range(nb)],
                      dma_sem=("pst", s_))
        S.add("sp", _dma(wgrp_b[:], wb[B_GRP, :, 0:512]), reads=[("scr", B_GRP)], writes=["wgrp_b"], dma_sem="rw0")
        S.add("sp", _dma(skT_b[:].rearrange("p (b n) -> p b n", b=2), wb[B_SK:B_SK + 2].rearrange("b p n -> p b n")),
              reads=[("scr", B_SK), ("scr", B_SK + 1)], writes=["skT_b"], dma_sem="rw1")

        ring_state = {"n": 0}

        def ring_next(blk):
            slot = ring_state["n"] % NB
            ring_state["n"] += 1
            key = ("ring", slot)
            S.add("sp", _dma(ring[:, slot, :], wb[blk]), reads=[("scr", blk)], writes=[key], dma_sem=("w", slot))
            return ring[:, slot, :], key

        mm_state = {}

        def mm_slot(banks=(4, 5)):
            i = mm_state.get(banks, 0)
            mm_state[banks] = i + 1
            bank = banks[i % len(banks)]
            return pb[bank][:, 0:256], bank

        sq_uni = (sq, lambda kc: [("sq", kc)], GK, ())
        sq_sc2 = (sc2[:].rearrange("p (m t) -> p m t", m=8), lambda kc: [("sc2", 2 * kc), ("sc2", 2 * kc + 1)], (), ())
        sq_eq = (eq[:].rearrange("p (m t) -> p m t", m=8), lambda kc: [("eqn", kc)], ("eq",), ("eq",))
        rs_std = (rstd, "rstd")
        rs_sif = (sif[:].rearrange("p a b -> p (a b)"), "sif")

        def rmsnorm_thunks(src, kin, dst, kout, gcol, out_after=(), sqs=None, rs=None):
            sqv, sqk, sq_after, sq_xr = sqs if sqs is not None else sq_uni
            rsv, rsk = rs if rs is not None else rs_std
            slot = {}

            def t1():
                for kc in range(8):
                    S.add("act", _act(sqv[:, kc, :], src[:, kc, :], AF.Square), reads=[(kin, kc)], writes=sqk(kc),
                          after=sq_after)

            def t2():
                ps_, pk = mm_slot()
                for kc in range(8):
                    S.add("pe", _mm(ps_, ones_f, sqv[:, kc, :], start=(kc == 0), stop=(kc == 7)),
                          reads=sqk(kc) + ["cst"] + list(sq_xr), psum=[pk])
                S.add("act", _act(rsv[:], ps_, AF.Sqrt, bias=EPS, scale=1.0 / 1024.0), psum=[pk], writes=[rsk])

            def t3():
                S.add("dve", lambda e: e.reciprocal(out=rsv[:], in_=rsv[:]), reads=[rsk], writes=[rsk])

            def t4():
                for kc in range(8):
                    S.add("dve", _stt(dst[:, kc, :], src[:, kc, :], cst[:, gcol + kc:gcol + kc + 1], rsv[:], ALU.mult,
                                      ALU.mult),
                          reads=[(kin, kc), rsk, "cst"], writes=[(kout, kc)], after=out_after)

            return [t1, t2, t3, t4]

        def rmsnorm(src, kin, dst, kout, gcol, out_after=(), sqs=None, rs=None):
            for t_ in rmsnorm_thunks(src, kin, dst, kout, gcol, out_after, sqs, rs):
                t_()

        S.add("sp", _dma(xT[0][:], xd[0]), writes=[("x0", kc) for kc in range(8)], dma_sem=("x", 0))
        def mixer(n, fin_thunks=()):
            fin_thunks = list(fin_thunks)
            par = n % 2
            npar = 1 - par
            first = (n % 8 == 0)
            X = xT[n % 3]
            xk = "x%d" % (n % 3)
            kTc, kTn = "kTc%d" % par, "kTc%d" % npar
            vc, vn = "vc%d" % par, "vc%d" % npar
            pzc, pzn = "pzc%d" % par, "pzc%d" % npar

            rmsnorm(X, xk, xnT, "xnT", C_G + 0, out_after=GK)

            def inproj_chunk(oc):
                wsl, wk = ring_next(B_WIN + oc)
                w3 = wsl.rearrange("p (k m) -> p k m", k=8)
                ps_, pk = mm_slot()
                for kc in range(8):
                    S.add("pe", _mm(ps_, w3[:, kc, :], xnT[:, kc, :], start=(kc == 0), stop=(kc == 7)),
                          reads=[wk, ("xnT", kc)], psum=[pk])
                bcol = cst[:, C_B + oc:C_B + oc + 1]
                if oc < 8:
                    S.add("act", _act(qT[:, oc, :], ps_, AF.Identity, bias=bcol), psum=[pk], reads=["cst"],
                          writes=[("qT", oc)], after=GK)
                elif oc < 10:
                    kv = oc - 8
                    S.add("act", _act(kT[:, par, kv, 128:384], ps_, AF.Identity, bias=bcol), psum=[pk], reads=["cst"],
                          writes=[kTc])
                    S.add("act", _act(kT[:, npar, kv, 0:128], ps_[:, 128:256], AF.Identity, bias=bcol),
                          psum=[pk], reads=["cst"], writes=[kTn])
                elif oc < 12:
                    kv = oc - 10
                    S.add("act", _act(vT[:, kv, :], ps_, AF.Identity, bias=bcol), psum=[pk], reads=["cst"],
                          writes=[("vT", kv)])
                    p3b = pb[3][:].bitcast(BF16)
                    for blk in range(2):
                        qi = kv * 2 + blk
                        o_ = p3b[:, qi * 256:qi * 256 + 128]
                        S.add("pe", _tr(o_, vT[:, kv, blk * 128:(blk + 1) * 128], identb[:]),
                              reads=[("vT", kv), "identb"], psum=[3])
                        S.add("dve", _cp(vlo[:, par, kv, blk + 1, 0:64], o_[:, 0:64]), psum=[3], writes=[vc])
                        S.add("dve", _cp(vhi[:, par, kv, blk + 1, 64:128], o_[:, 64:128]), psum=[3], writes=[vc])
                        if blk == 1:
                            S.add("dve", _cp(vlo[:, npar, kv, 0, 0:64], o_[:, 0:64]), psum=[3], writes=[vn])
                            S.add("dve", _cp(vhi[:, npar, kv, 0, 64:128], o_[:, 64:128]), psum=[3], writes=[vn])
                elif oc < 16:
                    g = oc - 12
                    if first:
                        S.add("pool", lambda e, g=g: e.memset(pz[:, par, g, 0:16], 0.0), writes=[("pz", par, g)])
                    S.add("act", _act(pz[:, par, g, 16:272], ps_, AF.Identity, bias=bcol), psum=[pk], reads=["cst"],
                          writes=[("pz", par, g)])
                    S.add("act", _act(pz[:, npar, g, 0:16], ps_[:, 240:256], AF.Identity, bias=bcol),
                          psum=[pk], reads=["cst"], writes=[("pz", npar, g)])
                elif oc < 24:
                    m = oc - 16
                    S.add("act", _act(sga[:, m, :], ps_, AF.Tanh, bias=bhalf[:, m:m + 1], scale=0.5), psum=[pk],
                          reads=["bhalf"], writes=[("sga", m)], after=GK)
                else:
                    m = oc - 24
                    S.add("act", _act(sgp[:, m, :], ps_, AF.Tanh, bias=bhalf[:, 8 + m:9 + m], scale=0.5), psum=[pk],
                          reads=["bhalf"], writes=[("sgp", m)], after=GK)

            for oc_ in range(12):
                inproj_chunk(oc_)
                if fin_thunks and oc_ % 2 == 0:
                    fin_thunks.pop(0)()
            while fin_thunks:
                fin_thunks.pop(0)()
            f_pool = [(lambda oc_=oc_: inproj_chunk(oc_)) for oc_ in range(12, 16)]

            def pool_group(g):
                w = 2 << g
                p_ = pz[:, par, g, :]
                cur = p_
                curk = [("pz", par, g)]
                sh = 1
                lvl = 0
                while sh < w:
                    dst = ptmp[:, lvl, :]
                    S.add("pool", _tt(dst[:, sh:272], cur[:, sh:272], cur[:, 0:272 - sh], ALU.add),
                          reads=curk, writes=[("ptmp", lvl)])
                    cur = dst
                    curk = [("ptmp", lvl)]
                    sh *= 2
                    lvl += 1
                S.add("dve", _stt(pooledT[:, g, :], cur[:, 16:272], 1.0 / w, p_[:, 16:272], ALU.mult, ALU.subtract),
                      reads=curk + [("pz", par, g)], writes=[("pooledT", g)])
                if first:
                    S.add("dve", _tt(pfix[:], cur[:, 16:32], cst[:, C_RC + g * 16:C_RC + (g + 1) * 16],
                                     ALU.mult), reads=curk + ["cst"], writes=["pfix"])
                    S.add("dve", _tt(pooledT[:, g, 0:16], pfix[:], p_[:, 16:32], ALU.subtract),
                          reads=["pfix", ("pz", par, g)], writes=[("pooledT", g)])
                ps_, pk = mm_slot()
                S.add("pe", _mm(ps_, wgrp_b[:, g * 128:(g + 1) * 128], pooledT[:, g, :]),
                      reads=["wgrp_b", ("pooledT", g)], psum=[pk])
                S.add("act", _act(ypT[:, g, :], ps_, AF.Identity, scale=cst[:, C_PSC + g:C_PSC + g + 1]),
                      psum=[pk], reads=["cst"], writes=[("ypT", g)])

            f_pool += [(lambda g_=g_: pool_group(g_)) for g_ in range(4)]
            f_gate = [(lambda oc_=oc_: inproj_chunk(oc_)) for oc_ in range(16, 32)]
            fillers = []
            for i_ in range(16):
                fillers.append(f_gate[i_])
                if i_ < 8:
                    fillers.append(f_pool[i_])

            def run_fillers(k):
                for _ in range(k):
                    if fillers:
                        fillers.pop(0)()

            def att_scores(c):
                kv = c // 4
                for hh in range(2):
                    bank = (c % 2) * 2 + hh
                    pr = slice(hh * 64, hh * 64 + 64)
                    ps_ = pb[bank]
                    rk = [("qT", c), kTc]
                    S.add("pe", _mm(ps_[:, 0:128], kT[pr, par, kv, 0:128], qT[pr, c, 0:128], start=True, stop=False,
                                    sgc=True), reads=rk, psum=[bank])
                    S.add("pe", _mm(ps_[:, 128:384], kT[pr, par, kv, 128:256], qT[pr, c, 0:256], start=False,
                                    stop=False, sgc=True), reads=rk, psum=[bank])
                    S.add("pe", _mm(ps_[:, 384:512], kT[pr, par, kv, 256:384], qT[pr, c, 128:256], start=False,
                                    stop=False, sgc=True), reads=rk, psum=[bank])
                    mk = maskb[:, 512:1024] if first else maskb[:, 0:512]
                    S.add("pe", _mm(ps_[:, 0:512], identb[:], mk, start=False, stop=True, sgc=True),
                          reads=["identb", "maskb"], psum=[bank])
                    eb = (c % 2) * 2 + hh
                    S.add("act", _act(PTb[:, eb, :], ps_[:], AF.Exp, scale=0.125), psum=[bank],
                          writes=["PT%d" % eb], after=GK)

            def att_pv(c):
                kv = c // 4
                ob = 6 + (c % 2)
                o_ps = pb[ob][:, 0:256]
                d_ps = pb[ob][:, 256:512]
                for blk in range(2):
                    seq = []
                    for hh in range(2):
                        eb = (c % 2) * 2 + hh
                        vv = vlo if hh == 0 else vhi
                        on = oneslh[:, hh * 128:(hh + 1) * 128]
                        for kb in range(2):
                            col = (blk * 2 + kb) * 128
                            seq.append((vv[:, par, kv, blk + kb, :], on, PTb[:, eb, col:col + 128], "PT%d" % eb))
                    for i_, (vw, on, rhs, rkey) in enumerate(seq):
                        S.add("pe", _mm(o_ps[:, blk * 128:(blk + 1) * 128], vw, rhs, start=(i_ == 0), stop=(i_ == 3)),
                              reads=[rkey, vc], psum=[ob])
                    for i_, (vw, on, rhs, rkey) in enumerate(seq):
                        S.add("pe", _mm(d_ps[:, blk * 128:(blk + 1) * 128], on, rhs, start=(i_ == 0), stop=(i_ == 3)),
                              reads=[rkey, "oneslh"], psum=[ob])
                r_ = c % 2
                S.add("dve", _ts(rd[:, r_, :], d_ps, sinkexp[:, c:c + 1], ALU.add), psum=[ob], reads=["sinkexp"],
                      writes=["rd%d" % r_], after=GK)
                S.add("dve", lambda e, r_=r_: e.reciprocal(out=rd[:, r_, :], in_=rd[:, r_, :]), reads=["rd%d" % r_],
                      writes=["rd%d" % r_])
                S.add("dve", _tt(attnT[:, c, :], o_ps, rd[:, r_, :], ALU.mult), psum=[ob], reads=["rd%d" % r_],
                      writes=[("attnT", c)], after=GK)

            att_scores(0)
            run_fillers(2)
            for c in range(8):
                if c + 1 < 8:
                    att_scores(c + 1)
                    run_fillers(2)
                att_pv(c)
                run_fillers(1)
            run_fillers(100)

            for m in range(8):
                wsl, wk = ring_next(B_AUP + m)
                w3 = wsl.rearrange("p (k m) -> p k m", k=8)
                ps_, pk = mm_slot()
                for kc in range(8):
                    S.add("pe", _mm(ps_, w3[:, kc, :], attnT[:, kc, :], start=(kc == 0), stop=(kc == 7)),
                          reads=[wk, ("attnT", kc)], psum=[pk])
                S.add("dve", _stt(tmpA[:, m, :], sga[:, m, :], 1.0, ps_, ALU.add, ALU.mult), psum=[pk], reads=[("sga", m)],
                      writes=[("tmpA", m)], after=GK)
            for mb in range(4):
                wsl, wk = ring_next(B_PUP + mb)
                w4 = wsl.rearrange("p (i k m) -> p i k m", i=2, k=4)
                for mi in range(2):
                    m = mb * 2 + mi
                    ps_, pk = mm_slot()
                    for kc in range(4):
                        S.add("pe", _mm(ps_, w4[:, mi, kc, :], ypT[:, kc, :], start=(kc == 0), stop=(kc == 3)),
                              reads=[wk, ("ypT", kc)], psum=[pk])
                    tb = m % 2
                    S.add("dve", _stt(tmpB[:, tb, :], sgp[:, m, :], 1.0, ps_, ALU.add, ALU.mult), psum=[pk], reads=[("sgp", m)],
                          writes=["tmpB%d" % tb], after=GK)
                    S.add("pool", _tt(mergedT[:, m, :], tmpB[:, tb, :], tmpA[:, m, :], ALU.add),
                          reads=["tmpB%d" % tb, ("tmpA", m)], writes=[("mergedT", m)], after=GK)
            for m in range(8):
                wsl, wk = ring_next(B_WO + m)
                w3 = wsl.rearrange("p (k m) -> p k m", k=8)
                ps_, pk = mm_slot()
                for kc in range(8):
                    S.add("pe", _mm(ps_, w3[:, kc, :], mergedT[:, kc, :], start=(kc == 0), stop=(kc == 7)),
                          reads=[wk, ("mergedT", kc)], psum=[pk])
                S.add("dve", _stt(X[:, m, :], ps_, 0.5, X[:, m, :], ALU.mult, ALU.add), psum=[pk], reads=[(xk, m)],
                      writes=[(xk, m)])
            if dbg:
                S.add("act", _dma(d_h2[n], X[:]), reads=[(xk, m) for m in range(8)], writes=["d_h2"],
                      dma_sem=("dbg", 0))

        def mixer_tail(n):
            par = n % 2
            X = xT[n % 3]
            xk = "x%d" % (n % 3)
            th = rmsnorm_thunks(X, xk, xn2[par], "xn2T%d" % par, C_G + 8, sqs=sq_sc2)

            def qchunk(oc):
                wsl, wk = ring_next(B_WQ + oc)
                w3 = wsl.rearrange("p (k m) -> p k m", k=8)
                ps_, pk = mm_slot()
                for kc in range(8):
                    S.add("pe", _mm(ps_, w3[:, kc, :], xn2[par][:, kc, :], start=(kc == 0), stop=(kc == 7)),
                          reads=[wk, ("xn2T%d" % par, kc)], psum=[pk])
                S.add("act", _act(qpT[:, oc, :], ps_, AF.Copy), psum=[pk], writes=[("qpT", oc)])

            th += [(lambda oc_=oc_: qchunk(oc_)) for oc_ in range(16)]
            return th

        def topk_q(n):
            QT = [_DQ(), _DQ()]
            QX = [_DQ(), _DQ()]
            for tt in range(2):
                tsl = slice(tt * 128, (tt + 1) * 128)
                Q = QT[tt]
                for b4_ in range(4):
                    bank = 6 + (b4_ % 2)
                    for i_ in range(4):
                        oc = b4_ * 4 + i_
                        Q.add("pe", _mm(pb[bank][:, i_ * 128:(i_ + 1) * 128], qpT[:, oc, tsl],
                                        skT_b[:, oc * 128:(oc + 1) * 128]),
                              reads=[("qpT", oc), "skT_b"], psum=[bank])
                    Q.add("act", _act(sc[:, b4_ * 512:(b4_ + 1) * 512], pb[bank][:], AF.Copy), psum=[bank],
                          writes=[("sc", b4_)])
                for gq in range(16):
                    Q.add("dve", lambda e, gq=gq: e.max(out=sv[:, gq, 0:8], in_=sc[:, gq * 128:(gq + 1) * 128]),
                          reads=[("sc", gq // 4)], writes=[("sv0", gq)])
                for gq in range(16):
                    Q.add("dve", lambda e, gq=gq: e.max_index(out=si[:, gq, 0:8], in_max=sv[:, gq, 0:8],
                                                              in_values=sc[:, gq * 128:(gq + 1) * 128]),
                          reads=[("sc", gq // 4), ("sv0", gq)], writes=[("si0", gq)])
                for gq in range(16):
                    Q.add("dve", lambda e, gq=gq: e.match_replace(out=sc2[:, gq * 128:(gq + 1) * 128],
                                                                  in_to_replace=sv[:, gq, 0:8],
                                                                  in_values=sc[:, gq * 128:(gq + 1) * 128],
                                                                  imm_value=NEG),
                          reads=[("sc", gq // 4), ("sv0", gq)], writes=[("sc2", gq)])
                for gq in range(16):
                    Q.add("dve", lambda e, gq=gq: e.max(out=sv[:, gq, 8:16], in_=sc2[:, gq * 128:(gq + 1) * 128]),
                          reads=[("sc2", gq)], writes=[("sv1", gq)])
                for gq in range(16):
                    Q.add("dve", lambda e, gq=gq: e.max_index(out=si[:, gq, 8:16], in_max=sv[:, gq, 8:16],
                                                              in_values=sc2[:, gq * 128:(gq + 1) * 128]),
                          reads=[("sc2", gq), ("sv1", gq)], writes=[("si1", gq)])
                allsv = [("sv0", gq) for gq in range(16)] + [("sv1", gq) for gq in range(16)]
                allsi = [("si0", gq) for gq in range(16)] + [("si1", gq) for gq in range(16)]
                Q.add("dve", _cp(sif[:], si[:]), reads=allsi, writes=["sif"])
                cand = sc
                sv4 = sv[:].rearrange("p (h two) k -> p h two k", two=2)
                c4 = cand[:].rearrange("p (h a b) -> p h a b", h=8, a=16)
                in0 = sv4[:, :, 0, :].unsqueeze(3).broadcast_to([128, 8, 16, 16])
                in1 = sv4[:, :, 1, :].unsqueeze(2).broadcast_to([128, 8, 16, 16])
                allsc = [("sc", b) for b in range(4)]
                allsc2 = [("sc2", gq) for gq in range(16)]
                Q.add("dve", _tt(c4, in0, in1, ALU.add), reads=allsv, writes=allsc)
                for h in range(8):
                    Q.add("dve", lambda e, h=h: e.max(out=fv[:, h, 0:8], in_=cand[:, h * 256:(h + 1) * 256]),
                          reads=[("sc", h // 2)], writes=[("fv0", h)])
                for h in range(8):
                    Q.add("dve", lambda e, h=h: e.max_index(out=fi[:, h, 0:8], in_max=fv[:, h, 0:8],
                                                            in_values=cand[:, h * 256:(h + 1) * 256]),
                          reads=[("sc", h // 2), ("fv0", h)], writes=[("fi0", h)])
                for h in range(8):
                    Q.add("dve", lambda e, h=h: e.match_replace(out=sc2[:, h * 256:(h + 1) * 256],
                                                                in_to_replace=fv[:, h, 0:8],
                                                                in_values=cand[:, h * 256:(h + 1) * 256],
                                                                imm_value=NEG),
                          reads=[("sc", h // 2), ("fv0", h)], writes=[("sc2", 2 * h), ("sc2", 2 * h + 1)])
                for h in range(8):
                    Q.add("dve", lambda e, h=h: e.max(out=fv[:, h, 8:16], in_=sc2[:, h * 256:(h + 1) * 256]),
                          reads=[("sc2", 2 * h), ("sc2", 2 * h + 1)], writes=[("fv1", h)])
                for h in range(8):
                    Q.add("dve", lambda e, h=h: e.max_index(out=fi[:, h, 8:16], in_max=fv[:, h, 8:16],
                                                            in_values=sc2[:, h * 256:(h + 1) * 256]),
                          reads=[("sc2", 2 * h), ("sc2", 2 * h + 1), ("fv1", h)], writes=[("fi1", h)])
                allfv = [("fv0", h) for h in range(8)] + [("fv1", h) for h in range(8)]
                allfi = [("fi0", h) for h in range(8)] + [("fi1", h) for h in range(8)]
                Q.add("dve", _tt(fsh[:], fv[:], fv[:, :, 0:1].broadcast_to([128, 8, 16]), ALU.subtract), reads=allfv,
                      writes=["fsh"])
                Q.add("act", _act(ex[:], fsh[:], AF.Exp), reads=["fsh"], writes=["ex"])
                Q.add("dve", lambda e: e.tensor_reduce(out=ssum[:], in_=ex[:], axis=AX.X, op=ALU.add), reads=["ex"],
                      writes=["ssum"])
                Q.add("dve", lambda e: e.reciprocal(out=ssum[:], in_=ssum[:]), reads=["ssum"], writes=["ssum"])
                gview = sel3[:, tt, 256:384].rearrange("p (h k) -> p h k", h=8)
                Q.add("dve", _tt(gview, ex[:], ssum[:].unsqueeze(2).broadcast_to([128, 8, 16]), ALU.mult),
                      reads=["ex", "ssum"], writes=[("sel_g", tt)])
                Q.add("dve", lambda e: e.tensor_single_scalar(out=au[:], in_=fi[:], scalar=4,
                                                              op=ALU.logical_shift_right), reads=allfi, writes=["au"])
                Q.add("dve", lambda e: e.tensor_single_scalar(out=bu[:], in_=fi[:], scalar=15, op=ALU.bitwise_and),
                      reads=allfi, writes=["bu"])
                Q.add("dve", _cp(af[:], au[:]), reads=["au"], writes=["af"])
                Q.add("dve", _cp(bf[:], bu[:]), reads=["bu"], writes=["bf"])
                sif4 = sif[:].rearrange("p (h two) k -> p h two k", two=2)
                eq4 = eq[:].rearrange("p (h k a) -> p h k a", h=8, k=16)
                io4 = iota_f[:, 0:16].unsqueeze(1).unsqueeze(1).broadcast_to([128, 8, 16, 16])
                for which, (srcf, half, off) in enumerate(((af, 0, 0), (bf, 1, 128))):
                    Q.add("dve", _tt(eq4, srcf[:].unsqueeze(3).broadcast_to([128, 8, 16, 16]), io4, ALU.is_equal),
                          reads=["af", "bf", "cst"], writes=["eq"])
                    Q.add("dve", _tt(eq4, eq4, sif4[:, :, half, :].unsqueeze(2).broadcast_to([128, 8, 16, 16]),
                                     ALU.mult), reads=["eq", "sif"], writes=["eq"])
                    Q.add("dve", lambda e, off=off, tt=tt: e.tensor_reduce(
                        out=sel3[:, tt, off:off + 128], in_=eq[:].rearrange("p (s a) -> p s a", a=16), axis=AX.X,
                        op=ALU.add), reads=["eq"], writes=[("sel_i%d" % which, tt)])
                selk = [("sel_g", tt), ("sel_i0", tt), ("sel_i1", tt)]
                if dbg:
                    Q.add("act", _dma(d_sel[n, tt], sel3[:, tt, :]), reads=selk, writes=["d_sel"], dma_sem=("dbg", 1))
                Q = QX[tt]
                for q3 in range(3):
                    Q.add("pe", _tr(pb[7][:, q3 * 128:(q3 + 1) * 128], sel3[:, tt, q3 * 128:(q3 + 1) * 128], ident_f),
                          reads=selk + ["cst"], psum=[7])
                Q.add("act", _act(selT[:, tt, :], pb[7][:, 0:384], AF.Copy), psum=[7], writes=[("selT", tt)])
            return QT, QX

        g_bank = {"n": 0}

        def gbuild(n):
            if 2 <= n + 2 < n_chunks:
                xi_ = (n + 2) % 3
                S.add("sp", _dma(xT[xi_][:], xd[n + 2]), writes=[("x%d" % xi_, kc) for kc in range(8)],
                      dma_sem=("x", xi_))
            th = []
            for tt in range(2):
                for t4 in range(32):
                    th.append(lambda tt=tt, t4=t4: gb_group(n, tt, t4))
            return th

        def gb_group(n, tt, t4):
            if True:
                if True:
                    gb = g_bank["n"] % 4
                    g_bank["n"] += 1
                    for tq in range(4):
                        tok = t4 * 4 + tq
                        slot = tok % NOH
                        S.add("dve", _ts(oh0[:, slot, :], iota_b[:], selT[:, tt, tok:tok + 1], ALU.is_equal,
                                         selT[:, tt, 256 + tok:256 + tok + 1], ALU.mult),
                              reads=[("selT", tt), "iota_b"], writes=[("oh0", slot)])
                        S.add("dve", _ts(oh1[:, slot, :], iota_b[:], selT[:, tt, 128 + tok:128 + tok + 1],
                                         ALU.is_equal),
                              reads=[("selT", tt), "iota_b"], writes=[("oh1", slot)])
                        S.add("pe", _mm(pb[gb][:, tq * 128:(tq + 1) * 128], oh0[:, slot, :], oh1[:, slot, :]),
                              reads=[("oh0", slot), ("oh1", slot)], psum=[gb])
                    gt0 = tt * 128 + t4 * 4
                    S.add("act", _act(G[:, gt0:gt0 + 4, :], pb[gb][:].rearrange("p (t j) -> p t j", t=4), AF.Copy),
                          psum=[gb], writes=["G"], after=UNI_KEYS if (tt == 0 and t4 == 0) else ())


        def jloop(n, tqs):
            par = n % 2
            npar = 1 - par
            first = (n % 8 == 0)
            X = xT[n % 3]
            xk = "x%d" % (n % 3)
            xn2T = xn2[par]
            xn2k = "xn2T%d" % par
            def peer_A(j):
                wsl, wk = ring_next(B_U + j)
                w3 = wsl.rearrange("p (k m) -> p k m", k=8)
                ps_, pk = mm_slot((4, 5))
                for kc in range(8):
                    S.add("pe", _mm(ps_, w3[:, kc, :], xn2T[:, kc, :], start=(kc == 0), stop=(kc == 7)),
                          reads=[wk, (xn2k, kc)], psum=[pk])
                b4 = j % NWB
                S.add("act", _act(ge[:, b4, :], ps_, AF.Gelu), psum=[pk], writes=[("ge", b4)])
                S.add("pool", _tt(wt[:, b4, :], ge[:, b4, :], G[:, :, j], ALU.mult), reads=[("ge", b4), "G"],
                      writes=[("wt", b4)])

            def peer_O(j):
                wsl, wk = ring_next(B_V + j)
                b4 = j % NWB
                for m in range(8):
                    S.add("pe", _mm(pb[m // 2][:, (m % 2) * 256:(m % 2 + 1) * 256], wsl[:, m * 128:(m + 1) * 128],
                                    wt[:, b4, :], start=(j == 0 and m % 2 == 0), stop=(j == 127), sgc=True),
                          reads=[wk, ("wt", b4)], psum=[m // 2])

            LAG = 4
            for j in range(128 + LAG):
                if j < 128:
                    peer_A(j)
                if j >= LAG:
                    peer_O(j - LAG)
                if tqs is not None:
                    qt_, qx_ = tqs
                    if j < 56:
                        qt_[0].replay(S, 3)
                    elif j < 64:
                        qt_[0].replay(S)
                        qt_[1].replay(S, 3)
                    elif j < 124:
                        qt_[1].replay(S, 3)
                        if j == 90:
                            qx_[0].replay(S)
                    else:
                        qt_[1].replay(S)
                        if j == 128 + LAG - 1:
                            qx_[1].replay(S)
            for m in range(8):
                S.add("dve", _tt(X[:, m, :], pb[m // 2][:, (m % 2) * 256:(m % 2 + 1) * 256], X[:, m, :], ALU.add),
                      psum=[m // 2], reads=[(xk, m)], writes=[(xk, m)])
            if dbg:
                S.add("act", _dma(d_h3[n], X[:]), reads=[(xk, m) for m in range(8)], writes=["d_h3"],
                      dma_sem=("dbg", 2))


        def final(n):
            par = n % 2
            npar = 1 - par
            first = (n % 8 == 0)
            X = xT[n % 3]
            xk = "x%d" % (n % 3)
            xn2T = xn2[par]
            xn2k = "xn2T%d" % par
            th = rmsnorm_thunks(X, xk, X, xk, C_G + 16, sqs=sq_eq, rs=rs_sif)

            def st():
                S.add("act", _dma(od[n], X[:]), reads=[(xk, m) for m in range(8)], writes=[("od", n % 3)] +
                      [(xk, m) for m in range(8)], dma_sem=("o", n % 3))

            return th + [st]

        if n_chunks > 1:
            S.add("sp", _dma(xT[1][:], xd[1]), writes=[("x1", kc) for kc in range(8)], dma_sem=("x", 1))
        mixer(0)
        for t_ in mixer_tail(0):
            t_()
        qt_, qx_ = topk_q(0)
        for i_ in range(2):
            qt_[i_].replay(S)
            qx_[i_].replay(S)
        pending_fin = []
        for n_ in range(n_chunks):
            tqs = None
            tail = []
            if n_ + 1 < n_chunks:
                mixer(n_ + 1, pending_fin)
                pending_fin = []
                tail = mixer_tail(n_ + 1)
                tqs = topk_q(n_ + 1)
            else:
                for t_ in pending_fin:
                    t_()
                pending_fin = []
            gbt = gbuild(n_)
            while gbt or tail:
                for _ in range(3):
                    if gbt:
                        gbt.pop(0)()
                if tail:
                    tail.pop(0)()
            jloop(n_, tqs)
            pending_fin = final(n_)
        for t_ in pending_fin:
            t_()
        last_out_keys = [("od", 0), ("od", 1), ("od", 2)]
        fin_reads = list(last_out_keys)
        if dbg:
            fin_reads += ["d_h2", "d_sel", "d_h3"]
        S.add("act", lambda e: e.nop(), reads=fin_reads)

        S.finalize()
        sems = {}
        for e in S.ENGS:
            sems[("eng", e)] = es.enter_context(nc.semaphore("s_" + e))
        for i, k in enumerate(S.dma_sems):
            sems[("dma", k)] = es.enter_context(nc.semaphore("d_%d" % i))
        with nc.Block() as block:
            @block.sync
            def _(eng):
                S.emit_engine("sp", eng, sems)

            @block.tensor
            def _(eng):
                S.emit_engine("pe", eng, sems)

            @block.scalar
            def _(eng):
                S.emit_engine("act", eng, sems)

            @block.vector
            def _(eng):
                S.emit_engine("dve", eng, sems)

            @block.gpsimd
            def _(eng):
                S.emit_engine("pool", eng, sems)
    return nc


def _blocks8(w):
    K, N = w.shape
    n = N // 128
    kk = K // 128
    return np.ascontiguousarray(w.reshape(kk, 128, n, 128).transpose(2, 1, 0, 3)).reshape(n, 128, kk * 128)


def prep_weights(inp):
    w_in = np.asarray(inp["w_in"][0], dtype=np.float32)
    b_in = np.asarray(inp["b_in"][0], dtype=np.float32)

    def cols(a):
        q, k, v = a[..., 0:1024], a[..., 1024:1152], a[..., 1152:1280]
        pzc, ga, gp = a[..., 1280:1792], a[..., 1792:2816], a[..., 2816:3840]
        k0, k1, v0, v1 = k[..., :64], k[..., 64:], v[..., :64], v[..., 64:]
        return np.concatenate([q, k0, k0, k1, k1, v0, v0, v1, v1, pzc, ga, gp], axis=-1)

    wcols = cols(w_in)
    bcols = cols(b_in)
    wf = np.zeros((NBLK, 128, 1024), dtype=np.float32)
    wf[B_WIN:B_WIN + 32] = _blocks8(wcols)
    wf[B_AUP:B_AUP + 8] = _blocks8(np.asarray(inp["w_attn_up"][0], dtype=np.float32))
    pu = _blocks8(np.asarray(inp["w_pool_up"][0], dtype=np.float32))
    wf[B_PUP:B_PUP + 4] = pu.reshape(4, 2, 128, 512).transpose(0, 2, 1, 3).reshape(4, 128, 1024)
    wf[B_WO:B_WO + 8] = _blocks8(np.asarray(inp["w_o"][0], dtype=np.float32))
    wf[B_WQ:B_WQ + 16] = _blocks8(np.asarray(inp["w_query"][0], dtype=np.float32))
    u = np.asarray(inp["u_experts"][0], dtype=np.float32)
    wf[B_U:B_U + 128] = u.reshape(128, 128, 8, 128).transpose(1, 3, 2, 0).reshape(128, 128, 1024)
    v = np.asarray(inp["v_experts"][0], dtype=np.float32)
    wf[B_V:B_V + 128] = v.reshape(128, 128, 1024).transpose(1, 0, 2)
    grp = np.asarray(inp["w_pool_grp"][0], dtype=np.float32)
    wf[B_GRP, :, 0:512] = grp.transpose(1, 0, 2).reshape(128, 512)
    sk = np.asarray(inp["sub_keys"][0], dtype=np.float32)
    skt = sk.transpose(3, 0, 1, 2).reshape(128, 2048)
    wf[B_SK] = skt[:, 0:1024]
    wf[B_SK + 1] = skt[:, 1024:2048]

    cst = np.zeros((128, NCST), dtype=np.float32)
    for i, nm in enumerate(("ln_mix_g", "ln_ffn_g", "ln_final_g")):
        gv = np.asarray(inp[nm], dtype=np.float32).reshape(-1)
        cst[:, C_G + 8 * i:C_G + 8 * i + 8] = gv.reshape(8, 128).T
    cst[:, C_B:C_B + 32] = bcols.reshape(32, 128).T
    sinks = np.asarray(inp["attn_sinks"][0], dtype=np.float32)
    for c in range(8):
        cst[0:64, C_SINK + c] = sinks[2 * c]
        cst[64:128, C_SINK + c] = sinks[2 * c + 1]
    cst[:, C_PSC:C_PSC + 4] = np.asarray(inp["pool_scale"][0], dtype=np.float32).reshape(4, 128).T
    cst[:, C_ID:C_ID + 128] = np.eye(128, dtype=np.float32)
    cst[:, C_ONES:C_ONES + 128] = 1.0
    cst[:, C_IOTA:C_IOTA + 128] = np.arange(128, dtype=np.float32)[None, :]
    kk = np.arange(128)[:, None]
    qq = np.arange(128)[None, :]
    MNEG = -30000.0
    prevm = np.where(qq < kk, 0.0, MNEG).astype(np.float32)
    diagm = np.where(qq >= kk, 0.0, MNEG).astype(np.float32)
    cst[:, C_MREST:C_MREST + 512] = np.concatenate([prevm, diagm, prevm, diagm], axis=1)
    cst[:, C_MFIRST:C_MFIRST + 512] = np.concatenate([0 * prevm + MNEG, diagm, prevm, diagm], axis=1)
    cst[:, C_OLO:C_OLO + 64] = 1.0
    cst[:, C_OHI + 64:C_OHI + 128] = 1.0
    for g in range(4):
        w = 2 << g
        cst[:, C_RC + g * 16:C_RC + (g + 1) * 16] = (1.0 / np.minimum(np.arange(1, 17), w)).astype(np.float32)[None]
    return wf, cst


def prep_x(x, core, n_chunks=NCH_FULL):
    xs = np.asarray(x[core * 4:(core + 1) * 4], dtype=np.float32).reshape(4 * 2048, 1024)[: n_chunks * T]
    return np.ascontiguousarray(xs.reshape(n_chunks, T, 8, 128).transpose(0, 3, 2, 1))


def unprep_out(od):
    n = od.shape[0]
    return np.ascontiguousarray(od.transpose(0, 3, 2, 1)).reshape(n * T, 1024)


def kernel(**inputs):
    wf, cst = prep_weights(inputs)
    x = inputs["x"]
    nc = build_nc(NCH_FULL)
    in_maps = [{"xd": prep_x(x, c), "wf": wf, "cst": cst} for c in range(N_CORES)]
    res = run_bass_kernel_spmd(nc, in_maps, core_ids=list(range(N_CORES)))
    out = np.concatenate([unprep_out(np.asarray(r["od"])) for r in res.results], axis=0)
    return out.reshape(32, 2048, 1024).astype(np.float32)
```
